# Optimizing a Trainium2 kernel written in Bass

```python
import math
import jax, jax.numpy as jnp
from jax import lax
import numpy as np

D_MODEL = 1024
BATCH = 16
SEQ = 2048
DEPTH = 1

GRID_W = 64
WIN_H = 8
WIN_W = 16
ATTN_HEADS = 8
HEAD_DIM = 64
ATTN_WIDTH = ATTN_HEADS * HEAD_DIM
CONV_WIDTH = D_MODEL - ATTN_WIDTH
CONV_K = 31
MIX_WIDTH = ATTN_WIDTH + CONV_WIDTH
IN_WIDTH = 3 * ATTN_WIDTH + 2 * CONV_WIDTH
PEER_HEADS = 8
PEER_QDIM = 256
PEER_HALF = PEER_QDIM // 2
N_KEYS = 128
N_EXPERTS = N_KEYS * N_KEYS
PEER_TOPK = 16
PEER_CHUNK = 128
LN_EPS = 1e-5
DEEPNORM_ALPHA = (2.0 * DEPTH) ** 0.25
DEEPNORM_BETA = (8.0 * DEPTH) ** -0.25
MASK_VALUE = -1e30

kernel_name = "hybrid_natten_conformer_peer_block"


def _layer_norm(x, gain=None, bias=None):
    xf = x.astype(jnp.float32)
    mu = jnp.mean(xf, axis=-1, keepdims=True)
    var = jnp.mean(jnp.square(xf - mu), axis=-1, keepdims=True)
    y = (xf - mu) * lax.rsqrt(var + LN_EPS)
    if gain is not None:
        y = y * gain.astype(jnp.float32) + bias.astype(jnp.float32)
    return y.astype(x.dtype)


def _neighbourhood_attention(q, k, v, rpb):
    b, s, h, dh = q.shape
    rows = s // GRID_W
    kh = min(WIN_H, rows)
    ncb = GRID_W // WIN_W
    kcw = 2 * WIN_W
    qg = q.reshape(b, rows, GRID_W, h, dh).transpose(1, 0, 3, 2, 4)
    kg = k.reshape(b, rows, GRID_W, h, dh).transpose(0, 3, 1, 2, 4)
    vg = v.reshape(b, rows, GRID_W, h, dh).transpose(0, 3, 1, 2, 4)
    qcol = jnp.arange(GRID_W)
    col_start = jnp.clip(qcol - WIN_W // 2, 0, GRID_W - WIN_W)
    blk_start = jnp.clip(jnp.arange(ncb) * WIN_W - WIN_W // 2, 0, GRID_W - kcw)
    key_col = blk_start[:, None] + jnp.arange(kcw)[None, :]
    qcol_b = qcol.reshape(ncb, WIN_W)
    cs_b = col_start.reshape(ncb, WIN_W)
    kc = key_col[:, None, :]
    col_valid = (kc >= cs_b[:, :, None]) & (kc < cs_b[:, :, None] + WIN_W)
    dc_idx = jnp.clip(kc - qcol_b[:, :, None] + WIN_W - 1, 0, 2 * WIN_W - 2)
    kg_c = kg[:, :, :, key_col]
    vg_c = vg[:, :, :, key_col]
    scale = dh ** -0.5

    def row_step(args):
        r, q_row = args
        rs = jnp.clip(r - kh // 2, 0, rows - kh)
        k_blk = lax.dynamic_slice_in_dim(kg_c, rs, kh, axis=2)
        v_blk = lax.dynamic_slice_in_dim(vg_c, rs, kh, axis=2)
        qb = q_row.reshape(b, h, ncb, WIN_W, dh)
        sc = jnp.einsum('bhnqd,bhinkd->bhnqik', qb, k_blk).astype(jnp.float32) * scale
        dr_idx = rs + jnp.arange(kh) - r + WIN_H - 1
        bias = rpb[:, dr_idx[None, None, :, None], dc_idx[:, :, None, :]]
        sc = sc + bias[None].astype(jnp.float32)
        sc = jnp.where(col_valid[:, :, None, :], sc, MASK_VALUE)
        p = jax.nn.softmax(sc, axis=(-2, -1))
        out = jnp.einsum('bhnqik,bhinkd->bhnqd', p.astype(v_blk.dtype), v_blk)
        return out.reshape(b, h, GRID_W, dh)

    out = lax.map(row_step, (jnp.arange(rows), qg))
    return out.transpose(1, 0, 3, 2, 4).reshape(b, s, h * dh)


def _conformer_conv(a, g, conv_w, conv_b, ln_g, ln_b):
    u = a * jax.nn.sigmoid(g)
    kern = conv_w[:, None, :].astype(u.dtype)
    y = lax.conv_general_dilated(u, kern, window_strides=(1,),
                                 padding=[(CONV_K // 2, CONV_K // 2)],
                                 dimension_numbers=('NWC', 'WIO', 'NWC'),
                                 feature_group_count=CONV_WIDTH) + conv_b
    y = _layer_norm(y, ln_g, ln_b)
    return jax.nn.silu(y)


def _peer(h, w_query, sub_keys, expert_u, expert_v):
    b, s, d = h.shape
    chunks = h.reshape((b * s) // PEER_CHUNK, PEER_CHUNK, d)

    def chunk_step(xc):
        t = xc.shape[0]
        q = (xc @ w_query).reshape(t, PEER_HEADS, 2, PEER_HALF)
        sc = jnp.einsum('thpc,hpnc->thpn', q, sub_keys).astype(jnp.float32)
        top_v, top_i = lax.top_k(sc, PEER_TOPK)
        cand = (top_v[:, :, 0, :, None] + top_v[:, :, 1, None, :]).reshape(t, PEER_HEADS, PEER_TOPK * PEER_TOPK)
        best_v, best_f = lax.top_k(cand, PEER_TOPK)
        i1 = jnp.take_along_axis(top_i[:, :, 0], best_f // PEER_TOPK, axis=-1)
        i2 = jnp.take_along_axis(top_i[:, :, 1], best_f % PEER_TOPK, axis=-1)
        idx = i1 * N_KEYS + i2
        gate = jax.nn.softmax(best_v, axis=-1)
        u = expert_u[idx]
        act = jax.nn.gelu(jnp.einsum('thkd,td->thk', u, xc).astype(jnp.float32), approximate=False)
        w = (gate * act).astype(xc.dtype)
        vv = expert_v[idx]
        return jnp.einsum('thk,thkd->td', w, vv)

    return lax.map(chunk_step, chunks).reshape(b, s, d)


def setup_inputs(seed: int = 0) -> dict:
    key = jax.random.key(seed)
    ks = jax.random.split(key, 24)
    L, D, A, C = DEPTH, D_MODEL, ATTN_WIDTH, CONV_WIDTH
    f32 = jnp.float32
    nrm = lambda k, shape, sc: jax.random.normal(k, shape, f32) * sc
    col_scale = jnp.concatenate([jnp.ones((2 * A,), f32), jnp.full((A,), DEEPNORM_BETA, f32),
                                 jnp.ones((2 * C,), f32)])
    return {
        "x": nrm(ks[0], (BATCH, SEQ, D), 1.0),
        "c": nrm(ks[1], (BATCH, D), 1.0),
        "w_ada": nrm(ks[2], (L, D, 6 * D), D ** -0.5),
        "b_ada": nrm(ks[3], (L, 6 * D), 0.01),
        "w_in": nrm(ks[4], (L, D, IN_WIDTH), D ** -0.5) * col_scale,
        "b_in": nrm(ks[5], (L, IN_WIDTH), 0.01),
        "rel_pos_bias": nrm(ks[6], (L, ATTN_HEADS, 2 * WIN_H - 1, 2 * WIN_W - 1), 0.1),
        "conv_w": nrm(ks[7], (L, CONV_K, C), CONV_K ** -0.5),
        "conv_b": nrm(ks[8], (L, C), 0.01),
        "conv_ln_g": 1.0 + nrm(ks[9], (L, C), 0.01),
        "conv_ln_b": nrm(ks[10], (L, C), 0.01),
        "w_out": nrm(ks[11], (L, MIX_WIDTH, D), MIX_WIDTH ** -0.5) * DEEPNORM_BETA,
        "b_out": nrm(ks[12], (L, D), 0.01),
        "ln1_g": 1.0 + nrm(ks[13], (L, D), 0.01),
        "ln1_b": nrm(ks[14], (L, D), 0.01),
        "w_query": nrm(ks[15], (L, D, PEER_HEADS * PEER_QDIM), D ** -0.5),
        "sub_keys": nrm(ks[16], (L, PEER_HEADS, 2, N_KEYS, PEER_HALF), PEER_HALF ** -0.5),
        "expert_u": nrm(ks[17], (L, N_EXPERTS, D), D ** -0.5),
        "expert_v": nrm(ks[18], (L, N_EXPERTS, D), DEEPNORM_BETA),
        "ln2_g": 1.0 + nrm(ks[19], (L, D), 0.01),
        "ln2_b": nrm(ks[20], (L, D), 0.01),
    }


def reference(x, c, w_ada, b_ada, w_in, b_in, rel_pos_bias, conv_w, conv_b, conv_ln_g, conv_ln_b,
              w_out, b_out, ln1_g, ln1_b, w_query, sub_keys, expert_u, expert_v, ln2_g, ln2_b):
    b, s, d = x.shape
    A = ATTN_WIDTH
    for l in range(DEPTH):
        mod = jax.nn.silu(c) @ w_ada[l] + b_ada[l]
        shift1, scale1, gate1, shift2, scale2, gate2 = [m[:, None, :] for m in jnp.split(mod, 6, axis=-1)]
        h = _layer_norm(x) * (1.0 + scale1) + shift1
        z = h @ w_in[l] + b_in[l]
        q = z[..., 0:A].reshape(b, s, ATTN_HEADS, HEAD_DIM)
        k = z[..., A:2 * A].reshape(b, s, ATTN_HEADS, HEAD_DIM)
        v = z[..., 2 * A:3 * A].reshape(b, s, ATTN_HEADS, HEAD_DIM)
        ca = z[..., 3 * A:3 * A + CONV_WIDTH]
        cg = z[..., 3 * A + CONV_WIDTH:]
        y_attn = _neighbourhood_attention(q, k, v, rel_pos_bias[l])
        y_conv = _conformer_conv(ca, cg, conv_w[l], conv_b[l], conv_ln_g[l], conv_ln_b[l])
        y = jnp.concatenate([y_attn, y_conv], axis=-1) @ w_out[l] + b_out[l]
        x = _layer_norm(DEEPNORM_ALPHA * x + gate1 * y, ln1_g[l], ln1_b[l])
        h = _layer_norm(x) * (1.0 + scale2) + shift2
        y = _peer(h, w_query[l], sub_keys[l], expert_u[l], expert_v[l])
        x = _layer_norm(DEEPNORM_ALPHA * x + gate2 * y, ln2_g[l], ln2_b[l])
    return x
```

```python
import numpy as np
from contextlib import ExitStack
import concourse.bass as bass
import concourse.mybir as mybir
from concourse.bass_utils import run_bass_kernel_spmd

F32 = mybir.dt.float32
BF16 = mybir.dt.bfloat16
I32 = mybir.dt.int32
U32 = mybir.dt.uint32
AF = mybir.ActivationFunctionType
ALU = mybir.AluOpType
AX = mybir.AxisListType

NCORES = 8
D = 1024
SEQ = 2048
NT = 16
NB = 2
NEXP = 16384
EPS = 1e-5
ALPHA = 2.0 ** 0.25
NEG = -1.0e4
NSLOT = 12
RING = 8


class T:
    def __init__(self, t, name):
        self.t = t
        self.name = name
        self.w = {}
        self.r = {}
        self.dsem = None
        self.dcnt = 0

    def __getitem__(self, k):
        return self.t[k]


class S:
    def __init__(self, nc, es):
        self.nc = nc
        self.es = es
        self.root = es
        self.eng = {'pe': nc.tensor, 'act': nc.scalar, 'dve': nc.vector, 'pool': nc.gpsimd, 'sp': nc.sync}
        self.sem = {k: es.enter_context(nc.semaphore('s_' + k)) for k in ('pe', 'act', 'dve', 'pool')}
        self.cnt = {k: 0 for k in self.sem}
        self.seen = {k: {} for k in self.eng}
        self.dts = []
        self.nsem = 4
        self.uid = 0

    def sb(self, name, shape, dt):
        self.uid += 1
        name = 't%d_%s' % (self.uid, name)
        return T(self.es.enter_context(self.nc.sbuf_tensor(name, list(shape), dt)), name)

    def ps(self, name, shape, dt):
        self.uid += 1
        name = 't%d_%s' % (self.uid, name)
        return T(self.es.enter_context(self.nc.psum_tensor(name, list(shape), dt)), name)

    def _wait(self, e, deps):
        for sid, (sem, val) in deps.items():
            if self.seen[e].get(sid, 0) < val:
                self.eng[e].wait_ge(sem, val)
                self.seen[e][sid] = val

    def _deps(self, e, reads, writes, skip=None):
        d = {}

        def add(m):
            for sid, tk in m.items():
                if sid == skip:
                    continue
                if sid not in d or d[sid][1] < tk[1]:
                    d[sid] = tk
        for t in reads:
            add(t.w)
        raw_self = d.get(id(self.sem[e])) if e in self.sem else None
        for t in writes:
            add(t.w)
            add(t.r)
        if e in self.sem:
            if raw_self is None or e == 'pe':
                d.pop(id(self.sem[e]), None)
            else:
                d[id(self.sem[e])] = raw_self
        return d

    def op(self, e, fn, reads=(), writes=()):
        self._wait(e, self._deps(e, reads, writes))
        ins = fn(self.eng[e])
        self.cnt[e] += 1
        sem = self.sem[e]
        tk = (sem, self.cnt[e])
        ins.then_inc(sem, 1)
        sid = id(sem)
        for t in reads:
            t.r[sid] = tk
        for t in writes:
            t.w = {sid: tk}
            t.r = {}
        return ins

    def dma(self, e, fn, tgt, reads=(), writes=()):
        if tgt.dsem is None:
            tgt.dsem = self.root.enter_context(self.nc.semaphore('d%d_' % self.nsem + tgt.name))
            self.nsem += 1
            self.dts.append(tgt)
        sid = id(tgt.dsem)
        skip = sid if (tgt in writes and not tgt.r) else None
        self._wait(e, self._deps(e, reads, writes, skip=skip))
        ins = fn(self.eng[e])
        tgt.dcnt += 16
        ins.then_inc(tgt.dsem, 16)
        tk = (tgt.dsem, tgt.dcnt)
        for t in reads:
            t.r[sid] = tk
        for t in writes:
            if t is tgt and skip is not None:
                t.w[sid] = tk
            else:
                t.w = {sid: tk}
            t.r = {}
        return ins

    def barrier(self):
        for e in self.eng:
            d = {}
            for k, sem in self.sem.items():
                if self.cnt[k]:
                    d[id(sem)] = (sem, self.cnt[k])
            for t in self.dts:
                d[id(t.dsem)] = (t.dsem, t.dcnt)
            self._wait(e, d)


def build(mode='full'):
    nc = bass.Bass("TRN2", target_bir_lowering=False)

    def din(name, shape, dt=F32):
        return nc.dram_tensor(name, list(shape), dt, kind="ExternalInput").ap()

    doA = mode in ('full', 'A')
    doB = mode in ('full', 'B', 'Bdbg', 'B1', 'B2', 'B4')
    b1 = mode in ('B1', 'B2', 'B4')
    nb1 = {'B1': 1, 'B2': 2, 'B4': 4}.get(mode, NT)
    dbg = mode == 'Bdbg'
    x_d = din("x", [NB, SEQ, D])
    cT_d = din("cT", [128, 8, NB])
    wada_d = din("w_ada", [D, 6 * D])
    bada_d = din("b_ada", [1, 6 * D])
    badafm_d = din("b_ada_fm", [128, 48])
    win_d = din("w_in", [D, 2560])
    binfm_d = din("b_in_fm", [128, 20])
    bin_d = din("b_in", [1, 2560])
    ebt_d = din("attn_bias", [128, 8, NSLOT * 128])
    cw_d = din("conv_w_fm", [128, 4, 31])
    cb_d = din("conv_b_fm", [128, 4])
    clg_d = din("conv_ln_g", [1, 512])
    clb_d = din("conv_ln_b", [1, 512])
    wout_d = din("w_out", [D, D])
    bout_d = din("b_out", [1, D])
    l1g_d = din("ln1_g", [1, D])
    l1b_d = din("ln1_b", [1, D])
    wq_d = din("w_query", [D, 2048])
    skT_d = din("skT", [128, 16, 128])
    euv_d = din("euv", [NEXP, 2 * D])
    euvb_d = nc.dram_tensor("euvb", [NEXP, 2 * D], BF16, kind="Internal").ap()
    l2g_d = din("ln2_g", [1, D])
    l2b_d = din("ln2_b", [1, D])
    ident_d = din("ident", [128, 128])
    iota_d = din("iota16", [128, 16])
    if mode == 'full':
        x1s_d = nc.dram_tensor("x1s", [NB, SEQ, D], F32, kind="Internal").ap()
    elif mode == 'A':
        x1s_d = nc.dram_tensor("x1s", [NB, SEQ, D], F32, kind="ExternalOutput").ap()
    else:
        x1s_d = din("x1s", [NB, SEQ, D])
    if dbg:
        dbg_idx = nc.dram_tensor("dbg_idx", [128, 128], I32, kind="ExternalOutput").ap()
        dbg_gate = nc.dram_tensor("dbg_gate", [128, 128], F32, kind="ExternalOutput").ap()
        dbg_h2 = nc.dram_tensor("dbg_h2", [128, D], F32, kind="ExternalOutput").ap()
        dbg_tv = nc.dram_tensor("dbg_tv", [128, 256], F32, kind="ExternalOutput").ap()
        dbg_ti = nc.dram_tensor("dbg_ti", [128, 256], U32, kind="ExternalOutput").ap()
        dbg_bv = nc.dram_tensor("dbg_bv", [128, 128], F32, kind="ExternalOutput").ap()
        dbg_bf = nc.dram_tensor("dbg_bf", [128, 128], U32, kind="ExternalOutput").ap()
    if doB:
        out_d = nc.dram_tensor("out", [NB, SEQ, D], F32, kind="ExternalOutput").ap()

    def bc(ap_row, n=128):
        return ap_row.partition_broadcast(n).rearrange("p a f -> p (a f)")

    with ExitStack() as es:
        s = S(nc, es)
        X1S = T(None, 'x1s')
        PS_S = [s.ps('ps_s0', [128, 1024], F32), s.ps('ps_s1', [128, 1024], F32)]
        PS_O = [s.ps('ps_o0', [128, 512], F32), s.ps('ps_o1', [128, 512], F32)]
        PS_M = s.ps('ps_m', [128, 512], F32)
        PS_T = s.ps('ps_t', [128, 1024], BF16)
        identf = s.sb('identf', [128, 128], F32)
        identb = s.sb('identb', [128, 128], BF16)
        iota16 = s.sb('iota16', [128, 16], F32)
        epst = s.sb('epst', [128, 1], F32)
        modfm = s.sb('modfm', [128, 48, NB], F32)
        gate1 = s.sb('gate1', [128, NB, D], F32)
        s.dma('sp', lambda e: e.dma_start(out=identf[:], in_=ident_d), identf, writes=[identf])
        s.dma('sp', lambda e: e.dma_start(out=iota16[:], in_=iota_d), iota16, writes=[iota16])
        s.op('dve', lambda e: e.tensor_copy(out=identb[:], in_=identf[:]), reads=[identf], writes=[identb])
        s.op('dve', lambda e: e.memset(epst[:], EPS), writes=[epst])

        def ln_stats(src_t, chunks, pfx, st, mv, rstd, nmr):
            for i, ap in enumerate(chunks):
                s.op('dve', lambda e, ap=ap, i=i: e.bn_stats(out=st[:, i, :], in_=ap), reads=[src_t], writes=[st])
            n = len(chunks)
            s.op('dve', lambda e: e.bn_aggr(out=mv[:], in_=st[:, 0:n, :]), reads=[st], writes=[mv])
            s.op('act', lambda e: e.activation(out=rstd[:], in_=mv[:, 1:2], func=AF.Sqrt, bias=epst[:], scale=1.0),
                 reads=[mv, epst], writes=[rstd])
            s.op('dve', lambda e: e.reciprocal(out=rstd[:], in_=rstd[:]), reads=[rstd], writes=[rstd])
            s.op('dve', lambda e: e.tensor_scalar(out=nmr[:], in0=mv[:, 0:1], scalar1=-1.0, scalar2=rstd[:],
                                                   op0=ALU.mult, op1=ALU.mult), reads=[mv, rstd], writes=[nmr])

        st_t = s.sb('st_t', [128, 2, 6], F32)
        mv_t = s.sb('mv_t', [128, 2], F32)
        rstd_t = s.sb('rstd_t', [128, 1], F32)
        nmr_t = s.sb('nmr_t', [128, 1], F32)

        def ada_phase(vecs_fm, vecs_bc, bcdst, root=None):
            with ExitStack() as es0:
                s.es = es0
                cT = s.sb('cT', [128, 8, NB], F32)
                sT = s.sb('sT', [128, 8, NB], F32)
                sTrep = s.sb('sTrep', [128, 8, NB, 128], F32)
                bfm = s.sb('bfm', [128, 48], F32)
                wst = [s.sb('wst0', [128, 8, 512], F32), s.sb('wst1', [128, 8, 512], F32)]
                bst = [s.sb('bst0', [128, 512], F32), s.sb('bst1', [128, 512], F32)]
                s.dma('sp', lambda e: e.dma_start(out=cT[:], in_=cT_d), cT, writes=[cT])
                s.dma('sp', lambda e: e.dma_start(out=bfm[:], in_=badafm_d), bfm, writes=[bfm])
                s.op('act', lambda e: e.activation(out=sT[:], in_=cT[:], func=AF.Silu), reads=[cT], writes=[sT])
                s.op('dve', lambda e: e.tensor_copy(out=sTrep[:], in_=sT[:, :, :, None].broadcast_to([128, 8, NB, 128])),
                     reads=[sT], writes=[sTrep])
                n = 0
                for blk in range(12):
                    v, half = blk // 2, blk % 2
                    if v not in vecs_fm and v not in vecs_bc:
                        continue
                    w = wst[n % 2]
                    bb = bst[n % 2]
                    n += 1
                    c0 = blk * 512
                    s.dma('sp', lambda e, w=w, c0=c0: e.dma_start(
                        out=w[:], in_=wada_d[:, c0:c0 + 512].rearrange("(k p) n -> p k n", p=128)), w, writes=[w])
                    if v in vecs_fm:
                        for j in range(4):
                            ch = blk * 4 + j
                            for k in range(8):
                                s.op('pe', lambda e, w=w, j=j, k=k: e.matmul(
                                    PS_M[:, j * 2:j * 2 + 2], lhsT=w[:, k, j * 128:(j + 1) * 128], rhs=sT[:, k, :],
                                    start=(k == 0), stop=(k == 7)), reads=[w, sT], writes=[PS_M])
                            add1 = 1.0 if v in (1, 4) else 0.0
                            s.op('dve', lambda e, j=j, ch=ch, add1=add1: e.tensor_scalar(
                                out=modfm[:, ch, :], in0=PS_M[:, j * 2:j * 2 + 2], scalar1=bfm[:, ch:ch + 1], scalar2=add1,
                                op0=ALU.add, op1=ALU.add), reads=[PS_M, bfm], writes=[modfm])
                    if v in vecs_bc:
                        s.dma('sp', lambda e, bb=bb, c0=c0: e.dma_start(out=bb[:], in_=bc(bada_d[:, c0:c0 + 512])),
                              bb, writes=[bb])
                        for b in range(NB):
                            P = PS_O[b]
                            for k in range(8):
                                s.op('pe', lambda e, w=w, b=b, k=k, P=P: e.matmul(
                                    P[:, :], lhsT=sTrep[:, k, b, :], rhs=w[:, k, :], start=(k == 0), stop=(k == 7)),
                                    reads=[w, sTrep], writes=[P])
                            dst = bcdst[v]
                            s.op('dve', lambda e, b=b, P=P, bb=bb, dst=dst, half=half: e.tensor_tensor(
                                out=dst[:, b, half * 512:(half + 1) * 512], in0=P[:, :], in1=bb[:], op=ALU.add),
                                reads=[P, bb], writes=[dst])
                            if v == 4:
                                s.op('dve', lambda e, b=b, dst=dst, half=half: e.tensor_scalar(
                                    out=dst[:, b, half * 512:(half + 1) * 512], in0=dst[:, b, half * 512:(half + 1) * 512],
                                    scalar1=1.0, scalar2=None, op0=ALU.add), reads=[dst], writes=[dst])
                s.barrier()
            s.es = root if root is not None else es

        if doA:
            ada_phase([0, 1], [2], {2: gate1})

        if doA:
            with ExitStack() as esA:
                s.es = esA
                wout = s.sb('wout', [128, 8, D], BF16)
                qT = s.sb('qT', [128, 4, SEQ], BF16)
                kT = s.sb('kT', [128, 4, SEQ], BF16)
                vaug = s.sb('vaug', [128, NT, 8, 65], BF16)
                upad = s.sb('upad', [128, 4, SEQ + 30], BF16)
                l1g = s.sb('l1g', [128, D], F32)
                l1b = s.sb('l1b', [128, D], F32)
                boutb = s.sb('boutb', [128, D], F32)
                clg = s.sb('clg', [128, 512], F32)
                clb = s.sb('clb', [128, 512], F32)
                bvb = s.sb('bvb', [128, 512], F32)
                cw = s.sb('cw', [128, 4, 31], F32)
                cb = s.sb('cb', [128, 4], F32)
                binfm = s.sb('binfm', [128, 20], F32)
                EB = s.sb('EB', [128, 8, NSLOT * 128], BF16)
                with ExitStack() as esE:
                    s.es = esE
                    ebsts = [s.sb('ebst0', [128, NSLOT * 128], F32), s.sb('ebst1', [128, NSLOT * 128], F32)]
                    for h in range(8):
                        eb_ = ebsts[h % 2]
                        s.dma('sp', lambda e, h=h, eb_=eb_: e.dma_start(out=eb_[:], in_=ebt_d[:, h, :]), eb_, writes=[eb_])
                        s.op('act', lambda e, h=h, eb_=eb_: e.activation(out=EB[:, h, :], in_=eb_[:], func=AF.Exp),
                             reads=[eb_], writes=[EB])
                    s.barrier()
                s.es = esA
                for k in range(8):
                    for hh in range(2):
                        s.dma('pool', lambda e, k=k, hh=hh: e.dma_start(
                            out=wout[:, k, hh * 512:(hh + 1) * 512], in_=wout_d[k * 128:(k + 1) * 128, hh * 512:(hh + 1) * 512]),
                            wout, writes=[wout])
                for dst, src in ((l1g, l1g_d), (l1b, l1b_d), (boutb, bout_d), (clg, clg_d), (clb, clb_d)):
                    s.dma('sp', lambda e, dst=dst, src=src: e.dma_start(out=dst[:], in_=bc(src)), dst, writes=[dst])
                s.dma('sp', lambda e: e.dma_start(out=bvb[:], in_=bc(bin_d[:, 1024:1536])), bvb, writes=[bvb])
                s.dma('sp', lambda e: e.dma_start(out=cw[:], in_=cw_d), cw, writes=[cw])
                s.dma('sp', lambda e: e.dma_start(out=cb[:], in_=cb_d), cb, writes=[cb])
                s.dma('sp', lambda e: e.dma_start(out=binfm[:], in_=binfm_d), binfm, writes=[binfm])
                s.op('pool', lambda e: e.memset(vaug[:], 1.0), writes=[vaug])
                s.op('pool', lambda e: e.memset(upad[:], 0.0), writes=[upad])

                for b in range(NB):
                    with ExitStack() as es1:
                        s.es = es1
                        win = s.sb('win', [128, 8, 2560], BF16)
                        xt = [s.sb('xt0', [128, D], F32), s.sb('xt1', [128, D], F32)]
                        xn = s.sb('xn', [128, D], BF16)
                        hT = s.sb('hT', [128, 8, 512], BF16)
                        sig = s.sb('sig', [128, 512], F32)
                        for k in range(8):
                            for cblk in range(5):
                                s.dma('pool', lambda e, k=k, cblk=cblk: e.dma_start(
                                    out=win[:, k, cblk * 512:(cblk + 1) * 512],
                                    in_=win_d[k * 128:(k + 1) * 128, cblk * 512:(cblk + 1) * 512]), win, writes=[win])
                        zi = 0
                        for stile in range(4):
                            for j in range(4):
                                i = stile * 4 + j
                                x_ = xt[i % 2]
                                s.dma('sp', lambda e, x_=x_, i=i: e.dma_start(out=x_[:], in_=x_d[b, i * 128:(i + 1) * 128, :]),
                                      x_, writes=[x_])
                                ln_stats(x_, [x_[:, 0:512], x_[:, 512:1024]], 'a1', st_t, mv_t, rstd_t, nmr_t)
                                s.op('act', lambda e, x_=x_: e.activation(out=xn[:], in_=x_[:], func=AF.Identity,
                                                                         bias=nmr_t[:], scale=rstd_t[:]),
                                     reads=[x_, nmr_t, rstd_t], writes=[xn])
                                for k in range(8):
                                    s.op('pe', lambda e, k=k: e.transpose(PS_T[:, k * 128:(k + 1) * 128],
                                                                          xn[:, k * 128:(k + 1) * 128], identb[:]),
                                         reads=[xn, identb], writes=[PS_T])
                                for k in range(8):
                                    s.op('dve', lambda e, k=k, j=j: e.tensor_scalar(
                                        out=hT[:, k, j * 128:(j + 1) * 128], in0=PS_T[:, k * 128:(k + 1) * 128],
                                        scalar1=modfm[:, 8 + k, b:b + 1], scalar2=modfm[:, k, b:b + 1],
                                        op0=ALU.mult, op1=ALU.add), reads=[PS_T, modfm], writes=[hT])
                            t0 = stile * 512
                            order = [('q', c) for c in range(4)] + [('k', c) for c in range(4)]
                            for c in range(4):
                                order += [('cg', c), ('ca', c)]
                            for kind, c in order:
                                col = {'q': 0, 'k': 512, 'ca': 1536, 'cg': 2048}[kind] + c * 128
                                ch = col // 128
                                P = PS_S[zi % 2]
                                zi += 1
                                for k in range(8):
                                    s.op('pe', lambda e, k=k, col=col, P=P: e.matmul(
                                        P[:, 0:512], lhsT=win[:, k, col:col + 128], rhs=hT[:, k, :],
                                        start=(k == 0), stop=(k == 7)), reads=[win, hT], writes=[P])
                                if kind in ('q', 'k'):
                                    dst = qT if kind == 'q' else kT
                                    s.op('act', lambda e, P=P, dst=dst, c=c, ch=ch: e.activation(
                                        out=dst[:, c, t0:t0 + 512], in_=P[:, 0:512], func=AF.Identity,
                                        bias=binfm[:, ch:ch + 1], scale=1.0), reads=[P, binfm], writes=[dst])
                                elif kind == 'cg':
                                    s.op('act', lambda e, P=P, ch=ch: e.activation(
                                        out=sig[:], in_=P[:, 0:512], func=AF.Sigmoid, bias=binfm[:, ch:ch + 1], scale=1.0),
                                        reads=[P, binfm], writes=[sig])
                                else:
                                    s.op('dve', lambda e, P=P, c=c, ch=ch: e.scalar_tensor_tensor(
                                        out=upad[:, c, 15 + t0:15 + t0 + 512], in0=P[:, 0:512], scalar=binfm[:, ch:ch + 1],
                                        in1=sig[:], op0=ALU.add, op1=ALU.mult), reads=[P, binfm, sig], writes=[upad])
                            for j in range(4):
                                i = stile * 4 + j
                                P = PS_S[zi % 2]
                                zi += 1
                                for k in range(8):
                                    s.op('pe', lambda e, k=k, j=j, P=P: e.matmul(
                                        P[:, 0:512], lhsT=hT[:, k, j * 128:(j + 1) * 128], rhs=win[:, k, 1024:1536],
                                        start=(k == 0), stop=(k == 7)), reads=[win, hT], writes=[P])
                                s.op('dve', lambda e, P=P, i=i: e.tensor_tensor(
                                    out=vaug[:, i, :, 0:64], in0=P[:, 0:512].rearrange("p (h d) -> p h d", d=64),
                                    in1=bvb[:].rearrange("p (h d) -> p h d", d=64), op=ALU.add),
                                    reads=[P, bvb], writes=[vaug])
                        s.barrier()
                    s.es = esA
                    with ExitStack() as es2:
                        s.es = es2
                        caccs = [s.sb('cacc%d' % c, [128, 512], F32) for c in range(4)]
                        Eb = [s.sb('E0', [128, 640], F32), s.sb('E1', [128, 640], F32)]
                        PT = [s.sb('PT0', [128, 640], BF16), s.sb('PT1', [128, 640], BF16)]
                        ymix = s.sb('ymix', [128, D], BF16)
                        yT = s.sb('yT', [128, 8, 128], BF16)
                        cn = s.sb('cn', [128, 512], F32)
                        rden = s.sb('rden', [128, 8], F32)
                        t1 = s.sb('t1', [128, D], F32)
                        x1t = [s.sb('x1t0', [128, D], F32), s.sb('x1t1', [128, D], F32)]
                        xr = [s.sb('xr0', [128, D], F32), s.sb('xr1', [128, D], F32)]
                        hc = 0
                        for stile in range(4):
                            t0 = stile * 512
                            for c in range(4):
                                s.op('dve', lambda e, c=c: e.tensor_scalar(
                                    out=caccs[c][:], in0=upad[:, c, t0:t0 + 512], scalar1=cw[:, c, 0:1], scalar2=cb[:, c:c + 1],
                                    op0=ALU.mult, op1=ALU.add), reads=[upad, cw, cb], writes=[caccs[c]])
                            for j in range(1, 31):
                                for c in range(4):
                                    s.op('dve', lambda e, c=c, j=j: e.scalar_tensor_tensor(
                                        out=caccs[c][:], in0=upad[:, c, t0 + j:t0 + j + 512], scalar=cw[:, c, j:j + 1],
                                        in1=caccs[c][:], op0=ALU.mult, op1=ALU.add), reads=[upad, cw, caccs[c]], writes=[caccs[c]])
                            for j in range(4):
                                i = stile * 4 + j
                                x_ = xr[i % 2]
                                s.dma('sp', lambda e, x_=x_, i=i: e.dma_start(out=x_[:], in_=x_d[b, i * 128:(i + 1) * 128, :]),
                                      x_, writes=[x_])
                                if i == 0:
                                    kts, slot0 = [0, 1, 2, 3], 3
                                elif i == 1:
                                    kts, slot0 = [0, 1, 2, 3], 2
                                elif i == NT - 2:
                                    kts, slot0 = [12, 13, 14, 15], 1
                                elif i == NT - 1:
                                    kts, slot0 = [12, 13, 14, 15], 0
                                else:
                                    kts, slot0 = [i - 2, i - 1, i, i + 1, i + 2], 7
                                nch = len(kts)
                                for h in range(8):
                                    c, pb = h // 2, (h % 2) * 64
                                    Sp = PS_S[hc % 2]
                                    E_ = Eb[hc % 2]
                                    P_ = PT[hc % 2]
                                    hc += 1
                                    for ci, kt in enumerate(kts):
                                        s.op('pe', lambda e, ci=ci, kt=kt, c=c, pb=pb, Sp=Sp: e.matmul(
                                            Sp[:, ci * 128:(ci + 1) * 128], lhsT=kT[pb:pb + 64, c, kt * 128:(kt + 1) * 128],
                                            rhs=qT[pb:pb + 64, c, i * 128:(i + 1) * 128], start=True, stop=True),
                                            reads=[kT, qT], writes=[Sp])
                                    s.op('act', lambda e, Sp=Sp, E_=E_: e.activation(
                                        out=E_[:, 0:nch * 128], in_=Sp[:, 0:nch * 128], func=AF.Exp, scale=0.125),
                                        reads=[Sp], writes=[E_])
                                    s.op('dve', lambda e, E_=E_, P_=P_, h=h: e.tensor_tensor(
                                        out=P_[:, 0:nch * 128], in0=E_[:, 0:nch * 128],
                                        in1=EB[:, h, slot0 * 128:(slot0 + nch) * 128], op=ALU.mult),
                                        reads=[E_, EB], writes=[P_])
                                    Po = PS_O[h // 4]
                                    o0 = (h % 4) * 65
                                    for ci, kt in enumerate(kts):
                                        s.op('pe', lambda e, ci=ci, kt=kt, P_=P_, Po=Po, o0=o0, h=h: e.matmul(
                                            Po[:, o0:o0 + 65], lhsT=P_[:, ci * 128:(ci + 1) * 128], rhs=vaug[:, kt, h, :],
                                            start=(ci == 0), stop=(ci == nch - 1)), reads=[P_, vaug], writes=[Po])
                                for g in range(2):
                                    Po = PS_O[g]
                                    ov = Po[:, 0:260].rearrange("p (h e) -> p h e", e=65)
                                    s.op('dve', lambda e, g=g, ov=ov: e.reciprocal(out=rden[:, g * 4:(g + 1) * 4], in_=ov[:, :, 64]),
                                         reads=[Po], writes=[rden])
                                    s.op('dve', lambda e, g=g, ov=ov: e.tensor_tensor(
                                        out=ymix[:, g * 256:(g + 1) * 256].rearrange("p (h d) -> p h d", d=64),
                                        in0=ov[:, :, 0:64],
                                        in1=rden[:, g * 4:(g + 1) * 4][:, :, None].broadcast_to([128, 4, 64]), op=ALU.mult),
                                        reads=[Po, rden], writes=[ymix])
                                for c in range(4):
                                    s.op('pe', lambda e, c=c, j=j: e.transpose(
                                        PS_M[:, c * 128:(c + 1) * 128], caccs[c][:, j * 128:(j + 1) * 128], identf[:]),
                                        reads=[caccs[c], identf], writes=[PS_M])
                                ln_stats(PS_M, [PS_M[:, 0:512]], 'cv', st_t, mv_t, rstd_t, nmr_t)
                                s.op('act', lambda e: e.activation(out=cn[:], in_=PS_M[:, :], func=AF.Identity,
                                                                   bias=nmr_t[:], scale=rstd_t[:]),
                                     reads=[PS_M, nmr_t, rstd_t], writes=[cn])
                                s.op('dve', lambda e: e.tensor_tensor(out=cn[:], in0=cn[:], in1=clg[:], op=ALU.mult),
                                     reads=[cn, clg], writes=[cn])
                                s.op('pool', lambda e: e.tensor_tensor(out=cn[:], in0=cn[:], in1=clb[:], op=ALU.add),
                                     reads=[cn, clb], writes=[cn])
                                s.op('act', lambda e: e.activation(out=ymix[:, 512:1024], in_=cn[:], func=AF.Silu),
                                     reads=[cn], writes=[ymix])
                                for k in range(8):
                                    s.op('pe', lambda e, k=k: e.transpose(PS_T[:, k * 128:(k + 1) * 128],
                                                                          ymix[:, k * 128:(k + 1) * 128], identb[:]),
                                         reads=[ymix, identb], writes=[PS_T])
                                s.op('act', lambda e: e.activation(out=yT[:].rearrange("p k t -> p (k t)"), in_=PS_T[:, :],
                                                                   func=AF.Copy), reads=[PS_T], writes=[yT])
                                for hh in range(2):
                                    for k in range(8):
                                        s.op('pe', lambda e, k=k, hh=hh: e.matmul(
                                            PS_M[:, :], lhsT=yT[:, k, :], rhs=wout[:, k, hh * 512:(hh + 1) * 512],
                                            start=(k == 0), stop=(k == 7)), reads=[yT, wout], writes=[PS_M])
                                    s.op('dve', lambda e, hh=hh: e.tensor_tensor(
                                        out=t1[:, hh * 512:(hh + 1) * 512], in0=PS_M[:, :], in1=boutb[:, hh * 512:(hh + 1) * 512],
                                        op=ALU.add), reads=[PS_M, boutb], writes=[t1])
                                s.op('pool', lambda e: e.tensor_tensor(out=t1[:], in0=t1[:], in1=gate1[:, b, :], op=ALU.mult),
                                     reads=[t1, gate1], writes=[t1])
                                s.op('dve', lambda e, x_=x_: e.scalar_tensor_tensor(
                                    out=t1[:], in0=x_[:], scalar=ALPHA, in1=t1[:], op0=ALU.mult, op1=ALU.add),
                                    reads=[x_, t1], writes=[t1])
                                ln_stats(t1, [t1[:, 0:512], t1[:, 512:1024]], 'l1', st_t, mv_t, rstd_t, nmr_t)
                                xo = x1t[i % 2]
                                s.op('act', lambda e, xo=xo: e.activation(out=xo[:], in_=t1[:], func=AF.Identity,
                                                                         bias=nmr_t[:], scale=rstd_t[:]),
                                     reads=[t1, nmr_t, rstd_t], writes=[xo])
                                s.op('dve', lambda e, xo=xo: e.tensor_tensor(out=xo[:], in0=xo[:], in1=l1g[:], op=ALU.mult),
                                     reads=[xo, l1g], writes=[xo])
                                s.op('pool', lambda e, xo=xo: e.tensor_tensor(out=xo[:], in0=xo[:], in1=l1b[:], op=ALU.add),
                                     reads=[xo, l1b], writes=[xo])
                                s.dma('sp', lambda e, xo=xo, i=i: e.dma_start(out=x1s_d[b, i * 128:(i + 1) * 128, :], in_=xo[:]),
                                      xo, reads=[xo], writes=[X1S])
                        s.barrier()
                    s.es = esA
            s.es = es

        if doB:
            esB = es.enter_context(ExitStack())
            s.es = esB
            gate2 = s.sb('gate2', [128, NB, D], F32)
            sh2 = s.sb('sh2', [128, NB, D], F32)
            sc2 = s.sb('sc2', [128, NB, D], F32)
            with ExitStack() as esC:
                s.es = esC
                cst = [s.sb('cst%d' % r, [128, 4, 2048], BF16) for r in range(4)]
                EUVB = T(None, 'euvb')
                euv_v = euv_d.rearrange("(p r) c -> p r c", p=128)
                euvb_v = euvb_d.rearrange("(p r) c -> p r c", p=128)

                def cast_store(j):
                    c_ = cst[j % 4]
                    s.dma('pool', lambda e: e.dma_start(out=euvb_v[:, 4 * j:4 * j + 4, :], in_=c_[:]), c_, reads=[c_])

                for j in range(32):
                    c_ = cst[j % 4]
                    s.dma('pool', lambda e, c_=c_, j=j: e.dma_start(out=c_[:], in_=euv_v[:, 4 * j:4 * j + 4, :]),
                          c_, writes=[c_])
                    if j >= 1:
                        cast_store(j - 1)
                cast_store(31)
                ada_phase([], [3, 4, 5], {3: sh2, 4: sc2, 5: gate2}, root=esC)
                s.es = esC
                s.barrier()
            s.es = esB
            if True:
                wq = s.sb('wq', [128, 8, 2048], BF16)
                skT = s.sb('skT', [128, 16, 128], BF16)
                l2g = s.sb('l2g', [128, D], F32)
                l2b = s.sb('l2b', [128, D], F32)
                G = [s.sb('G%d' % r, [128, 2 * D], BF16) for r in range(RING)]
                x1t = [s.sb('bx0', [128, D], F32), s.sb('bx1', [128, D], F32)]
                h2 = [s.sb('h2a', [128, D], F32), s.sb('h2b_', [128, D], F32)]
                idx = [s.sb('idxa', [128, 128], I32), s.sb('idxb', [128, 128], I32)]
                gate = [s.sb('gatea', [128, 128], F32), s.sb('gateb', [128, 128], F32)]
                h2b = s.sb('h2b', [128, D], BF16)
                hT2 = s.sb('hT2', [128, 8, 128], BF16)
                qT2 = s.sb('qT2', [128, 16, 128], BF16)
                tmps = [s.sb('tmpa', [128, 128], F32), s.sb('tmpb', [128, 128], F32)]
                tmp2s = [s.sb('tmp2a', [128, 256], F32), s.sb('tmp2b', [128, 256], F32)]
                tv = s.sb('tv', [128, 16, 16], F32)
                ti = s.sb('ti', [128, 16, 16], U32)
                tif = s.sb('tif', [128, 16, 16], F32)
                cand = s.sb('cand', [128, 8, 16, 16], F32)
                bigs = [s.sb('biga', [128, 8, 16, 16], F32), s.sb('bigb', [128, 8, 16, 16], F32)]
                bv = s.sb('bv', [128, 8, 16], F32)
                bf = s.sb('bf', [128, 8, 16], U32)
                ais = [s.sb('aia', [128, 8, 16], U32), s.sb('aib', [128, 8, 16], U32)]
                afs = [s.sb('afa', [128, 8, 16], F32), s.sb('afb', [128, 8, 16], F32)]
                i1 = s.sb('i1', [128, 8, 16], F32)
                i2 = s.sb('i2', [128, 8, 16], F32)
                eg = s.sb('eg', [128, 8, 16], F32)
                zs = s.sb('zs', [128, 8], F32)
                actv = s.sb('actv', [128, 128], F32)
                gl = s.sb('gl', [128, 128], F32)
                wcol = s.sb('wcol', [128, 128], F32)
                tvw = [T(tv.t, 'tvw0'), T(tv.t, 'tvw1')]
                tiw = [T(ti.t, 'tiw0'), T(ti.t, 'tiw1')]
                bvw = [T(bv.t, 'bvw0'), T(bv.t, 'bvw1')]
                bfw = [T(bf.t, 'bfw0'), T(bf.t, 'bfw1')]
                actv_w = [T(actv.t, 'actv_w%d' % k) for k in range(4)]
                gl_w = [T(gl.t, 'gl_w%d' % k) for k in range(4)]
                wcol_w = [T(wcol.t, 'wcol_w%d' % k) for k in range(4)]
                dg = [s.sb('dg%d' % r, [128, 128], BF16) for r in range(3)]
                junk = s.sb('junk', [128, D], BF16)
                acc = s.sb('acc', [128, D], F32)
                ot = [s.sb('ot0', [128, D], F32), s.sb('ot1', [128, D], F32)]
                for k in range(8):
                    for cblk in range(4):
                        s.dma('pool', lambda e, k=k, cblk=cblk: e.dma_start(
                            out=wq[:, k, cblk * 512:(cblk + 1) * 512],
                            in_=wq_d[k * 128:(k + 1) * 128, cblk * 512:(cblk + 1) * 512]), wq, writes=[wq])
                for g4 in range(4):
                    s.dma('pool', lambda e, g4=g4: e.dma_start(out=skT[:, g4 * 4:(g4 + 1) * 4, :], in_=skT_d[:, g4 * 4:(g4 + 1) * 4, :]),
                          skT, writes=[skT])
                s.dma('sp', lambda e: e.dma_start(out=l2g[:], in_=bc(l2g_d)), l2g, writes=[l2g])
                s.dma('sp', lambda e: e.dma_start(out=l2b[:], in_=bc(l2b_d)), l2b, writes=[l2b])
                st_b = s.sb('st_b', [128, 2, 6], F32)
                mv_b = s.sb('mv_b', [128, 2], F32)
                rstd_b = s.sb('rstd_b', [128, 1], F32)
                nmr_b = s.sb('nmr_b', [128, 1], F32)
                PSC = PS_S[0]
                PACC = PS_S[1]

                def route(b, i, p):
                    x_, h2_, idx_, gate_ = x1t[p], h2[p], idx[p], gate[p]
                    s.dma('sp', lambda e: e.dma_start(out=x_[:], in_=x1s_d[b, i * 128:(i + 1) * 128, :]),
                          x_, reads=[X1S], writes=[x_])
                    yield 3
                    for ci_, ap_ in enumerate([x_[:, 0:512], x_[:, 512:1024]]):
                        s.op('dve', lambda e, ci_=ci_, ap_=ap_: e.bn_stats(out=st_b[:, ci_, :], in_=ap_), reads=[x_], writes=[st_b])
                    s.op('dve', lambda e: e.bn_aggr(out=mv_b[:], in_=st_b[:, 0:2, :]), reads=[st_b], writes=[mv_b])
                    s.op('act', lambda e: e.activation(out=rstd_b[:], in_=mv_b[:, 1:2], func=AF.Sqrt, bias=epst[:], scale=1.0),
                         reads=[mv_b, epst], writes=[rstd_b])
                    yield 2
                    s.op('dve', lambda e: e.reciprocal(out=rstd_b[:], in_=rstd_b[:]), reads=[rstd_b], writes=[rstd_b])
                    yield 1
                    s.op('dve', lambda e: e.tensor_scalar(out=nmr_b[:], in0=mv_b[:, 0:1], scalar1=-1.0, scalar2=rstd_b[:],
                                                           op0=ALU.mult, op1=ALU.mult), reads=[mv_b, rstd_b], writes=[nmr_b])
                    s.op('act', lambda e: e.activation(out=h2_[:], in_=x_[:], func=AF.Identity, bias=nmr_b[:], scale=rstd_b[:]),
                         reads=[x_, nmr_b, rstd_b], writes=[h2_])
                    yield 2
                    s.op('dve', lambda e: e.tensor_tensor(out=h2_[:], in0=h2_[:], in1=sc2[:, b, :], op=ALU.mult),
                         reads=[h2_, sc2], writes=[h2_])
                    yield
                    s.op('dve', lambda e: e.tensor_tensor(out=h2_[:], in0=h2_[:], in1=sh2[:, b, :], op=ALU.add),
                         reads=[h2_, sh2], writes=[h2_])
                    yield
                    s.op('act', lambda e: e.activation(out=h2b[:], in_=h2_[:], func=AF.Copy), reads=[h2_], writes=[h2b])
                    for k in range(8):
                        s.op('pe', lambda e, k=k: e.transpose(PS_T[:, k * 128:(k + 1) * 128], h2b[:, k * 128:(k + 1) * 128], identb[:]),
                             reads=[h2b, identb], writes=[PS_T])
                    yield
                    s.op('act', lambda e: e.activation(out=hT2[:].rearrange("p k t -> p (k t)"), in_=PS_T[:, :], func=AF.Copy),
                         reads=[PS_T], writes=[hT2])
                    yield
                    for g4 in range(4):
                        P = PS_O[g4 % 2]
                        for cc in range(4):
                            ch = g4 * 4 + cc
                            for k in range(8):
                                s.op('pe', lambda e, k=k, ch=ch, cc=cc, P=P: e.matmul(
                                    P[:, cc * 128:(cc + 1) * 128], lhsT=wq[:, k, ch * 128:(ch + 1) * 128], rhs=hT2[:, k, :],
                                    start=(k == 0), stop=(k == 7)), reads=[wq, hT2], writes=[P])
                            yield
                        s.op('act', lambda e, g4=g4, P=P: e.activation(
                            out=qT2[:, g4 * 4:(g4 + 1) * 4, :].rearrange("p c t -> p (c t)"), in_=P[:, :], func=AF.Copy),
                            reads=[P], writes=[qT2])
                        yield
                    for half8 in range(2):
                        for q8 in range(8):
                            hp = half8 * 8 + q8
                            s.op('pe', lambda e, hp=hp, q8=q8: e.matmul(
                                PSC[:, q8 * 128:(q8 + 1) * 128], lhsT=qT2[:, hp, :], rhs=skT[:, hp, :], start=True, stop=True),
                                reads=[qT2, skT], writes=[PSC])
                        yield (16 if half8 == 0 else 3)
                        for q8 in range(0, 8, 2):
                            hps = [half8 * 8 + q8, half8 * 8 + q8 + 1]
                            srcs = [PSC[:, q8 * 128:(q8 + 1) * 128], PSC[:, (q8 + 1) * 128:(q8 + 2) * 128]]
                            for hp, src in zip(hps, srcs):
                                s.op('dve', lambda e, hp=hp, src=src: e.max(out=tv[:, hp, 0:8], in_=src), reads=[PSC], writes=[tvw[hp % 2]])
                                yield
                            for hp, src, tm in zip(hps, srcs, tmps):
                                s.op('dve', lambda e, hp=hp, src=src, tm=tm: e.match_replace(
                                    out=tm[:], in_to_replace=tv[:, hp, 0:8], in_values=src, imm_value=-1e30),
                                    reads=[PSC, tvw[hp % 2]], writes=[tm])
                                yield
                            for hp, tm in zip(hps, tmps):
                                s.op('dve', lambda e, hp=hp, tm=tm: e.max(out=tv[:, hp, 8:16], in_=tm[:]), reads=[tm], writes=[tvw[hp % 2]])
                                yield
                            for hp, src in zip(hps, srcs):
                                s.op('dve', lambda e, hp=hp, src=src: e.max_index(out=ti[:, hp, 0:8], in_max=tv[:, hp, 0:8],
                                                                                 in_values=src), reads=[PSC, tvw[hp % 2]], writes=[tiw[hp % 2]])
                                yield
                            for hp, tm in zip(hps, tmps):
                                s.op('dve', lambda e, hp=hp, tm=tm: e.max_index(out=ti[:, hp, 8:16], in_max=tv[:, hp, 8:16],
                                                                               in_values=tm[:]), reads=[tm, tvw[hp % 2]], writes=[tiw[hp % 2]])
                                yield
                    tv4 = tv[:].rearrange("p (h t) k -> p h t k", t=2)
                    s.op('dve', lambda e: e.tensor_tensor(
                        out=cand[:], in0=tv4[:, :, 0, :][:, :, :, None].broadcast_to([128, 8, 16, 16]),
                        in1=tv4[:, :, 1, :][:, :, None, :].broadcast_to([128, 8, 16, 16]), op=ALU.add),
                        reads=tvw, writes=[cand])
                    yield
                    for h0 in range(0, 8, 2):
                        hs = [h0, h0 + 1]
                        cvs = [cand[:, h, :, :].rearrange("p a b -> p (a b)") for h in hs]
                        for h, cv in zip(hs, cvs):
                            s.op('dve', lambda e, h=h, cv=cv: e.max(out=bv[:, h, 0:8], in_=cv), reads=[cand], writes=[bvw[h % 2]])
                            yield
                        for h, cv, tm in zip(hs, cvs, tmp2s):
                            s.op('dve', lambda e, h=h, cv=cv, tm=tm: e.match_replace(
                                out=tm[:], in_to_replace=bv[:, h, 0:8], in_values=cv, imm_value=-1e30),
                                reads=[cand, bvw[h % 2]], writes=[tm])
                            yield
                        for h, tm in zip(hs, tmp2s):
                            s.op('dve', lambda e, h=h, tm=tm: e.max(out=bv[:, h, 8:16], in_=tm[:]), reads=[tm], writes=[bvw[h % 2]])
                            yield
                        for h, cv in zip(hs, cvs):
                            s.op('dve', lambda e, h=h, cv=cv: e.max_index(out=bf[:, h, 0:8], in_max=bv[:, h, 0:8], in_values=cv),
                                 reads=[cand, bvw[h % 2]], writes=[bfw[h % 2]])
                            yield
                        for h, tm in zip(hs, tmp2s):
                            s.op('dve', lambda e, h=h, tm=tm: e.max_index(out=bf[:, h, 8:16], in_max=bv[:, h, 8:16], in_values=tm[:]),
                                 reads=[tm, bvw[h % 2]], writes=[bfw[h % 2]])
                            yield
                    s.op('dve', lambda e: e.tensor_copy(out=tif[:], in_=ti[:]), reads=tiw, writes=[tif])
                    yield
                    tif4 = tif[:].rearrange("p (h t) k -> p h t k", t=2)
                    s.op('dve', lambda e: e.tensor_scalar(out=ais[0][:], in0=bf[:], scalar1=4, scalar2=None,
                                                           op0=ALU.logical_shift_right), reads=bfw, writes=[ais[0]])
                    s.op('dve', lambda e: e.tensor_scalar(out=ais[1][:], in0=bf[:], scalar1=15, scalar2=None,
                                                           op0=ALU.bitwise_and), reads=bfw, writes=[ais[1]])
                    yield
                    for half in range(2):
                        s.op('dve', lambda e, half=half: e.tensor_copy(out=afs[half][:], in_=ais[half][:]),
                             reads=[ais[half]], writes=[afs[half]])
                    yield
                    for half in range(2):
                        s.op('dve', lambda e, half=half: e.tensor_tensor(
                            out=bigs[half][:], in0=afs[half][:][:, :, :, None].broadcast_to([128, 8, 16, 16]),
                            in1=iota16[:][:, None, None, :].broadcast_to([128, 8, 16, 16]), op=ALU.is_equal),
                            reads=[afs[half], iota16], writes=[bigs[half]])
                        yield
                    for half in range(2):
                        s.op('dve', lambda e, half=half: e.tensor_tensor(
                            out=bigs[half][:], in0=bigs[half][:],
                            in1=tif4[:, :, half, :][:, :, None, :].broadcast_to([128, 8, 16, 16]),
                            op=ALU.mult), reads=[bigs[half], tif], writes=[bigs[half]])
                        yield
                    for half, dst in ((0, i1), (1, i2)):
                        s.op('dve', lambda e, half=half, dst=dst: e.tensor_reduce(out=dst[:], in_=bigs[half][:], axis=AX.X, op=ALU.add),
                             reads=[bigs[half]], writes=[dst])
                        yield
                    s.op('dve', lambda e: e.scalar_tensor_tensor(
                        out=idx_[:].rearrange("p (h k) -> p h k", k=16), in0=i1[:], scalar=128.0, in1=i2[:],
                        op0=ALU.mult, op1=ALU.add), reads=[i1, i2], writes=[idx_])
                    s.op('dve', lambda e: e.tensor_tensor(out=eg[:], in0=bv[:], in1=bv[:, :, 0:1].broadcast_to([128, 8, 16]),
                                                           op=ALU.subtract), reads=bvw, writes=[eg])
                    yield
                    s.op('act', lambda e: e.activation(out=eg[:], in_=eg[:], func=AF.Exp), reads=[eg], writes=[eg])
                    yield 2
                    s.op('dve', lambda e: e.tensor_reduce(out=zs[:], in_=eg[:], axis=AX.X, op=ALU.add), reads=[eg], writes=[zs])
                    yield 1
                    s.op('dve', lambda e: e.reciprocal(out=zs[:], in_=zs[:]), reads=[zs], writes=[zs])
                    s.op('dve', lambda e: e.tensor_tensor(
                        out=gate_[:].rearrange("p (h k) -> p h k", k=16), in0=eg[:],
                        in1=zs[:][:, :, None].broadcast_to([128, 8, 16]), op=ALU.mult), reads=[eg, zs], writes=[gate_])
                    yield

                idle = [0]

                def drain(g, n=None):
                    if g is None:
                        return None
                    if n is not None and idle[0] > 0:
                        idle[0] -= 1
                        return g
                    k = 0
                    for v in g:
                        k += 1
                        if n is not None and v:
                            idle[0] = int(v) - 1
                            return g
                        if n is not None and k >= n:
                            return g
                    return None

                tiles = [(b, i) for b in range(1 if (dbg or b1) else NB) for i in range(nb1)]
                drain(route(tiles[0][0], tiles[0][1], 0))
                gi = 0
                for tn, (b, i) in enumerate(tiles):
                    p = tn % 2
                    x_, h2_, idx_, gate_ = x1t[p], h2[p], idx[p], gate[p]
                    if dbg:
                        s.barrier()
                        for dd, tt, vw in ((dbg_idx, idx_, idx_[:]), (dbg_gate, gate_, gate_[:]), (dbg_h2, h2_, h2_[:]),
                                           (dbg_tv, tv, tv[:].rearrange("p a b -> p (a b)")),
                                           (dbg_ti, ti, ti[:].rearrange("p a b -> p (a b)")),
                                           (dbg_bv, bv, bv[:].rearrange("p a b -> p (a b)")),
                                           (dbg_bf, bf, bf[:].rearrange("p a b -> p (a b)"))):
                            s.dma('sp', lambda e, dd=dd, vw=vw: e.dma_start(out=dd, in_=vw), tt, reads=[tt])
                        break
                    nxt = route(tiles[tn + 1][0], tiles[tn + 1][1], 1 - p) if tn + 1 < len(tiles) else None

                    def vstep(sl, g_):
                        d_ = dg[sl % 3]
                        s.op('act', lambda e: e.activation(out=wcol[:, sl:sl + 1], in_=gl[:, sl:sl + 1], func=AF.Copy,
                                                           scale=gate_[:, sl:sl + 1]), reads=[gl_w[sl % 4], gate_], writes=[wcol_w[sl % 4]])
                        s.op('act', lambda e: e.activation(out=d_[:], in_=identb[:], func=AF.Copy, scale=wcol[:, sl:sl + 1]),
                             reads=[identb, wcol_w[sl % 4]], writes=[d_])
                        for hh in range(2):
                            s.op('pe', lambda e, hh=hh: e.matmul(
                                PACC[:, hh * 512:(hh + 1) * 512], lhsT=d_[:], rhs=g_[:, D + hh * 512:D + (hh + 1) * 512],
                                start=(sl == 0), stop=(sl == 127)), reads=[d_, g_], writes=[PACC])

                    prev = None
                    for sl in range(128):
                        g_ = G[gi % RING]
                        gi += 1
                        s.dma('pool', lambda e, g_=g_, sl=sl: e.indirect_dma_start(
                            out=g_[:], out_offset=None, in_=euvb_d,
                            in_offset=bass.IndirectOffsetOnAxis(ap=idx_[:, sl:sl + 1], axis=0)), g_, reads=[idx_], writes=[g_])
                        s.op('dve', lambda e, g_=g_, sl=sl: e.scalar_tensor_tensor(
                            out=junk[:], in0=g_[:, 0:D], scalar=1.0, in1=h2_[:], op0=ALU.mult, op1=ALU.mult,
                            accum_out=actv[:, sl:sl + 1]), reads=[g_, h2_], writes=[actv_w[sl % 4]])
                        if prev is not None:
                            vstep(*prev)
                        s.op('act', lambda e, sl=sl: e.activation(out=gl[:, sl:sl + 1], in_=actv[:, sl:sl + 1], func=AF.Gelu),
                             reads=[actv_w[sl % 4]], writes=[gl_w[sl % 4]])
                        prev = (sl, g_)
                        nxt = drain(nxt, 3)
                    vstep(*prev)
                    drain(nxt)
                    for hh in range(2):
                        s.op('dve', lambda e, hh=hh: e.tensor_tensor(
                            out=acc[:, hh * 512:(hh + 1) * 512], in0=PACC[:, hh * 512:(hh + 1) * 512],
                            in1=gate2[:, b, hh * 512:(hh + 1) * 512], op=ALU.mult), reads=[PACC, gate2], writes=[acc])
                    s.op('dve', lambda e, x_=x_: e.scalar_tensor_tensor(
                        out=acc[:], in0=x_[:], scalar=ALPHA, in1=acc[:], op0=ALU.mult, op1=ALU.add),
                        reads=[x_, acc], writes=[acc])
                    ln_stats(acc, [acc[:, 0:512], acc[:, 512:1024]], 'l2', st_t, mv_t, rstd_t, nmr_t)
                    o_ = ot[tn % 2]
                    s.op('act', lambda e, o_=o_: e.activation(out=o_[:], in_=acc[:], func=AF.Identity,
                                                             bias=nmr_t[:], scale=rstd_t[:]),
                         reads=[acc, nmr_t, rstd_t], writes=[o_])
                    s.op('dve', lambda e, o_=o_: e.tensor_tensor(out=o_[:], in0=o_[:], in1=l2g[:], op=ALU.mult),
                         reads=[o_, l2g], writes=[o_])
                    s.op('dve', lambda e, o_=o_: e.tensor_tensor(out=o_[:], in0=o_[:], in1=l2b[:], op=ALU.add),
                         reads=[o_, l2b], writes=[o_])
                    s.dma('sp', lambda e, o_=o_, i=i, b=b: e.dma_start(out=out_d[b, i * 128:(i + 1) * 128, :], in_=o_[:]),
                          o_, reads=[o_])
                s.barrier()
            s.es = es
        s.barrier()
    return nc


def _bias_table(rpb):
    specs = [(d, 'F') for d in (-6, -4, -2, 0, 2, 4, 6)] + [(-4, 'I'), (-2, 'F'), (0, 'F'), (2, 'F'), (4, 'I')]
    kk = np.arange(128)
    kro, kc = kk // 64, kk % 64
    qro, qc = kk // 64, kk % 64
    cs = np.clip(qc - 8, 0, 48)
    colv = (kc[:, None] >= cs[None, :]) & (kc[:, None] < cs[None, :] + 16)
    dc = np.clip(kc[:, None] - qc[None, :] + 15, 0, 30)
    tab = np.empty((8, NSLOT, 128, 128), np.float32)
    for si, (dl, kind) in enumerate(specs):
        dr = dl + kro[:, None] - qro[None, :]
        valid = colv.copy()
        if kind == 'I':
            valid &= (dr >= -4) & (dr <= 3)
        dri = np.clip(dr + 7, 0, 14)
        vals = rpb[:, dri, dc]
        tab[:, si] = np.where(valid[None], vals, np.float32(NEG))
    return np.ascontiguousarray(tab.transpose(2, 0, 1, 3).reshape(128, 8, NSLOT * 128))


def make_in_maps(inp):
    f = lambda a: np.ascontiguousarray(np.asarray(a, dtype=np.float32))
    x = f(inp["x"])
    c = f(inp["c"])
    shared = {
        "w_ada": f(inp["w_ada"][0]),
        "b_ada": f(inp["b_ada"][0]).reshape(1, -1),
        "b_ada_fm": f(f(inp["b_ada"][0]).reshape(48, 128).T),
        "w_in": f(inp["w_in"][0]),
        "b_in_fm": f(f(inp["b_in"][0]).reshape(20, 128).T),
        "b_in": f(inp["b_in"][0]).reshape(1, -1),
        "attn_bias": _bias_table(f(inp["rel_pos_bias"][0])),
        "conv_w_fm": f(f(inp["conv_w"][0]).reshape(31, 4, 128).transpose(2, 1, 0)),
        "conv_b_fm": f(f(inp["conv_b"][0]).reshape(4, 128).T),
        "conv_ln_g": f(inp["conv_ln_g"][0]).reshape(1, -1),
        "conv_ln_b": f(inp["conv_ln_b"][0]).reshape(1, -1),
        "w_out": f(inp["w_out"][0]),
        "b_out": f(inp["b_out"][0]).reshape(1, -1),
        "ln1_g": f(inp["ln1_g"][0]).reshape(1, -1),
        "ln1_b": f(inp["ln1_b"][0]).reshape(1, -1),
        "w_query": f(inp["w_query"][0]),
        "skT": f(f(inp["sub_keys"][0]).reshape(16, 128, 128).transpose(2, 0, 1)),
        "euv": np.ascontiguousarray(np.concatenate([f(inp["expert_u"][0]), f(inp["expert_v"][0])], axis=1)),
        "ln2_g": f(inp["ln2_g"][0]).reshape(1, -1),
        "ln2_b": f(inp["ln2_b"][0]).reshape(1, -1),
        "ident": np.eye(128, dtype=np.float32),
        "iota16": np.ascontiguousarray(np.broadcast_to(np.arange(16, dtype=np.float32), (128, 16))),
    }
    maps = []
    for core in range(NCORES):
        m = dict(shared)
        m["x"] = np.ascontiguousarray(x[NB * core:NB * (core + 1)])
        m["cT"] = np.ascontiguousarray(c[NB * core:NB * (core + 1)].reshape(NB, 8, 128).transpose(2, 1, 0))
        maps.append(m)
    return maps


_NC_CACHE = {}


def kernel(**inputs):
    if 'full' not in _NC_CACHE:
        _NC_CACHE['full'] = build('full')
    nc = _NC_CACHE['full']
    maps = make_in_maps(inputs)
    res = run_bass_kernel_spmd(nc, maps, core_ids=list(range(NCORES)))
    out = np.concatenate([r["out"] for r in res.results], axis=0)
    return out.astype(np.float32)
```

```python
import numpy as np
from contextlib import ExitStack
import concourse.bass as bass
import concourse.mybir as mybir
from concourse.bass_utils import run_bass_kernel_spmd

F32 = mybir.dt.float32
BF16 = mybir.dt.bfloat16
I32 = mybir.dt.int32
U32 = mybir.dt.uint32
AF = mybir.ActivationFunctionType
ALU = mybir.AluOpType
AX = mybir.AxisListType

NCORES = 8
D = 1024
SEQ = 2048
NT = 16
NB = 2
NEXP = 16384
EPS = 1e-5
ALPHA = 2.0 ** 0.25
NEG = -1.0e4
NSLOT = 12
RING = 8


class T:
    def __init__(self, t, name):
        self.t = t
        self.name = name
        self.w = {}
        self.r = {}
        self.dsem = None
        self.dcnt = 0

    def __getitem__(self, k):
        return self.t[k]


class S:
    def __init__(self, nc, es):
        self.nc = nc
        self.es = es
        self.root = es
        self.eng = {'pe': nc.tensor, 'act': nc.scalar, 'dve': nc.vector, 'pool': nc.gpsimd, 'sp': nc.sync}
        self.sem = {k: es.enter_context(nc.semaphore('s_' + k)) for k in ('pe', 'act', 'dve', 'pool')}
        self.cnt = {k: 0 for k in self.sem}
        self.seen = {k: {} for k in self.eng}
        self.dts = []
        self.nsem = 4
        self.uid = 0

    def sb(self, name, shape, dt):
        self.uid += 1
        name = 't%d_%s' % (self.uid, name)
        return T(self.es.enter_context(self.nc.sbuf_tensor(name, list(shape), dt)), name)

    def ps(self, name, shape, dt):
        self.uid += 1
        name = 't%d_%s' % (self.uid, name)
        return T(self.es.enter_context(self.nc.psum_tensor(name, list(shape), dt)), name)

    def _wait(self, e, deps):
        for sid, (sem, val) in deps.items():
            if self.seen[e].get(sid, 0) < val:
                self.eng[e].wait_ge(sem, val)
                self.seen[e][sid] = val

    def _deps(self, e, reads, writes, skip=None):
        d = {}

        def add(m):
            for sid, tk in m.items():
                if sid == skip:
                    continue
                if sid not in d or d[sid][1] < tk[1]:
                    d[sid] = tk
        for t in reads:
            add(t.w)
        raw_self = d.get(id(self.sem[e])) if e in self.sem else None
        for t in writes:
            add(t.w)
            add(t.r)
        if e in self.sem:
            if raw_self is None or e == 'pe':
                d.pop(id(self.sem[e]), None)
            else:
                d[id(self.sem[e])] = raw_self
        return d

    def op(self, e, fn, reads=(), writes=()):
        self._wait(e, self._deps(e, reads, writes))
        ins = fn(self.eng[e])
        self.cnt[e] += 1
        sem = self.sem[e]
        tk = (sem, self.cnt[e])
        ins.then_inc(sem, 1)
        sid = id(sem)
        for t in reads:
            t.r[sid] = tk
        for t in writes:
            t.w = {sid: tk}
            t.r = {}
        return ins

    def dma(self, e, fn, tgt, reads=(), writes=()):
        if tgt.dsem is None:
            tgt.dsem = self.root.enter_context(self.nc.semaphore('d%d_' % self.nsem + tgt.name))
            self.nsem += 1
            self.dts.append(tgt)
        sid = id(tgt.dsem)
        skip = sid if (tgt in writes and not tgt.r) else None
        self._wait(e, self._deps(e, reads, writes, skip=skip))
        ins = fn(self.eng[e])
        tgt.dcnt += 16
        ins.then_inc(tgt.dsem, 16)
        tk = (tgt.dsem, tgt.dcnt)
        for t in reads:
            t.r[sid] = tk
        for t in writes:
            if t is tgt and skip is not None:
                t.w[sid] = tk
            else:
                t.w = {sid: tk}
            t.r = {}
        return ins

    def barrier(self):
        for e in self.eng:
            d = {}
            for k, sem in self.sem.items():
                if self.cnt[k]:
                    d[id(sem)] = (sem, self.cnt[k])
            for t in self.dts:
                d[id(t.dsem)] = (t.dsem, t.dcnt)
            self._wait(e, d)


def build(mode='full'):
    nc = bass.Bass("TRN2", target_bir_lowering=False)

    def din(name, shape, dt=F32):
        return nc.dram_tensor(name, list(shape), dt, kind="ExternalInput").ap()

    doA = mode in ('full', 'A')
    doB = mode in ('full', 'B', 'Bdbg', 'B1', 'B2', 'B4')
    b1 = mode in ('B1', 'B2', 'B4')
    nb1 = {'B1': 1, 'B2': 2, 'B4': 4}.get(mode, NT)
    dbg = mode == 'Bdbg'
    x_d = din("x", [NB, SEQ, D])
    cT_d = din("cT", [128, 8, NB])
    wada_d = din("w_ada", [D, 6 * D])
    bada_d = din("b_ada", [1, 6 * D])
    badafm_d = din("b_ada_fm", [128, 48])
    win_d = din("w_in", [D, 2560])
    binfm_d = din("b_in_fm", [128, 20])
    bin_d = din("b_in", [1, 2560])
    ebt_d = din("attn_bias", [128, 8, NSLOT * 128])
    cw_d = din("conv_w_fm", [128, 4, 31])
    cb_d = din("conv_b_fm", [128, 4])
    clg_d = din("conv_ln_g", [1, 512])
    clb_d = din("conv_ln_b", [1, 512])
    wout_d = din("w_out", [D, D])
    bout_d = din("b_out", [1, D])
    l1g_d = din("ln1_g", [1, D])
    l1b_d = din("ln1_b", [1, D])
    wq_d = din("w_query", [D, 2048])
    skT_d = din("skT", [128, 16, 128])
    euv_d = din("euv", [NEXP, 2 * D])
    euvb_d = nc.dram_tensor("euvb", [NEXP, 2 * D], BF16, kind="Internal").ap()
    l2g_d = din("ln2_g", [1, D])
    l2b_d = din("ln2_b", [1, D])
    ident_d = din("ident", [128, 128])
    iota_d = din("iota16", [128, 16])
    if mode == 'full':
        x1s_d = nc.dram_tensor("x1s", [NB, SEQ, D], F32, kind="Internal").ap()
    elif mode == 'A':
        x1s_d = nc.dram_tensor("x1s", [NB, SEQ, D], F32, kind="ExternalOutput").ap()
    else:
        x1s_d = din("x1s", [NB, SEQ, D])
    if dbg:
        dbg_idx = nc.dram_tensor("dbg_idx", [128, 128], I32, kind="ExternalOutput").ap()
        dbg_gate = nc.dram_tensor("dbg_gate", [128, 128], F32, kind="ExternalOutput").ap()
        dbg_h2 = nc.dram_tensor("dbg_h2", [128, D], F32, kind="ExternalOutput").ap()
        dbg_tv = nc.dram_tensor("dbg_tv", [128, 256], F32, kind="ExternalOutput").ap()
        dbg_ti = nc.dram_tensor("dbg_ti", [128, 256], U32, kind="ExternalOutput").ap()
        dbg_bv = nc.dram_tensor("dbg_bv", [128, 128], F32, kind="ExternalOutput").ap()
        dbg_bf = nc.dram_tensor("dbg_bf", [128, 128], U32, kind="ExternalOutput").ap()
    if doB:
        out_d = nc.dram_tensor("out", [NB, SEQ, D], F32, kind="ExternalOutput").ap()

    def bc(ap_row, n=128):
        return ap_row.partition_broadcast(n).rearrange("p a f -> p (a f)")

    with ExitStack() as es:
        s = S(nc, es)
        X1S = T(None, 'x1s')
        PS_S = [s.ps('ps_s0', [128, 1024], F32), s.ps('ps_s1', [128, 1024], F32)]
        PS_O = [s.ps('ps_o0', [128, 512], F32), s.ps('ps_o1', [128, 512], F32)]
        PS_M = s.ps('ps_m', [128, 512], F32)
        PS_T = s.ps('ps_t', [128, 1024], BF16)
        identf = s.sb('identf', [128, 128], F32)
        identb = s.sb('identb', [128, 128], BF16)
        iota16 = s.sb('iota16', [128, 16], F32)
        epst = s.sb('epst', [128, 1], F32)
        modfm = s.sb('modfm', [128, 48, NB], F32)
        gate1 = s.sb('gate1', [128, NB, D], F32)
        s.dma('sp', lambda e: e.dma_start(out=identf[:], in_=ident_d), identf, writes=[identf])
        s.dma('sp', lambda e: e.dma_start(out=iota16[:], in_=iota_d), iota16, writes=[iota16])
        s.op('dve', lambda e: e.tensor_copy(out=identb[:], in_=identf[:]), reads=[identf], writes=[identb])
        s.op('dve', lambda e: e.memset(epst[:], EPS), writes=[epst])

        def ln_stats(src_t, chunks, pfx, st, mv, rstd, nmr):
            for i, ap in enumerate(chunks):
                s.op('dve', lambda e, ap=ap, i=i: e.bn_stats(out=st[:, i, :], in_=ap), reads=[src_t], writes=[st])
            n = len(chunks)
            s.op('dve', lambda e: e.bn_aggr(out=mv[:], in_=st[:, 0:n, :]), reads=[st], writes=[mv])
            s.op('act', lambda e: e.activation(out=rstd[:], in_=mv[:, 1:2], func=AF.Sqrt, bias=epst[:], scale=1.0),
                 reads=[mv, epst], writes=[rstd])
            s.op('dve', lambda e: e.reciprocal(out=rstd[:], in_=rstd[:]), reads=[rstd], writes=[rstd])
            s.op('dve', lambda e: e.tensor_scalar(out=nmr[:], in0=mv[:, 0:1], scalar1=-1.0, scalar2=rstd[:],
                                                   op0=ALU.mult, op1=ALU.mult), reads=[mv, rstd], writes=[nmr])

        st_t = s.sb('st_t', [128, 2, 6], F32)
        mv_t = s.sb('mv_t', [128, 2], F32)
        rstd_t = s.sb('rstd_t', [128, 1], F32)
        nmr_t = s.sb('nmr_t', [128, 1], F32)

        def ada_phase(vecs_fm, vecs_bc, bcdst, root=None):
            with ExitStack() as es0:
                s.es = es0
                cT = s.sb('cT', [128, 8, NB], F32)
                sT = s.sb('sT', [128, 8, NB], F32)
                sTrep = s.sb('sTrep', [128, 8, NB, 128], F32)
                bfm = s.sb('bfm', [128, 48], F32)
                wst = [s.sb('wst0', [128, 8, 512], F32), s.sb('wst1', [128, 8, 512], F32)]
                bst = [s.sb('bst0', [128, 512], F32), s.sb('bst1', [128, 512], F32)]
                s.dma('sp', lambda e: e.dma_start(out=cT[:], in_=cT_d), cT, writes=[cT])
                s.dma('sp', lambda e: e.dma_start(out=bfm[:], in_=badafm_d), bfm, writes=[bfm])
                s.op('act', lambda e: e.activation(out=sT[:], in_=cT[:], func=AF.Silu), reads=[cT], writes=[sT])
                s.op('dve', lambda e: e.tensor_copy(out=sTrep[:], in_=sT[:, :, :, None].broadcast_to([128, 8, NB, 128])),
                     reads=[sT], writes=[sTrep])
                n = 0
                for blk in range(12):
                    v, half = blk // 2, blk % 2
                    if v not in vecs_fm and v not in vecs_bc:
                        continue
                    w = wst[n % 2]
                    bb = bst[n % 2]
                    n += 1
                    c0 = blk * 512
                    s.dma('sp', lambda e, w=w, c0=c0: e.dma_start(
                        out=w[:], in_=wada_d[:, c0:c0 + 512].rearrange("(k p) n -> p k n", p=128)), w, writes=[w])
                    if v in vecs_fm:
                        for j in range(4):
                            ch = blk * 4 + j
                            for k in range(8):
                                s.op('pe', lambda e, w=w, j=j, k=k: e.matmul(
                                    PS_M[:, j * 2:j * 2 + 2], lhsT=w[:, k, j * 128:(j + 1) * 128], rhs=sT[:, k, :],
                                    start=(k == 0), stop=(k == 7)), reads=[w, sT], writes=[PS_M])
                            add1 = 1.0 if v in (1, 4) else 0.0
                            s.op('dve', lambda e, j=j, ch=ch, add1=add1: e.tensor_scalar(
                                out=modfm[:, ch, :], in0=PS_M[:, j * 2:j * 2 + 2], scalar1=bfm[:, ch:ch + 1], scalar2=add1,
                                op0=ALU.add, op1=ALU.add), reads=[PS_M, bfm], writes=[modfm])
                    if v in vecs_bc:
                        s.dma('sp', lambda e, bb=bb, c0=c0: e.dma_start(out=bb[:], in_=bc(bada_d[:, c0:c0 + 512])),
                              bb, writes=[bb])
                        for b in range(NB):
                            P = PS_O[b]
                            for k in range(8):
                                s.op('pe', lambda e, w=w, b=b, k=k, P=P: e.matmul(
                                    P[:, :], lhsT=sTrep[:, k, b, :], rhs=w[:, k, :], start=(k == 0), stop=(k == 7)),
                                    reads=[w, sTrep], writes=[P])
                            dst = bcdst[v]
                            s.op('dve', lambda e, b=b, P=P, bb=bb, dst=dst, half=half: e.tensor_tensor(
                                out=dst[:, b, half * 512:(half + 1) * 512], in0=P[:, :], in1=bb[:], op=ALU.add),
                                reads=[P, bb], writes=[dst])
                            if v == 4:
                                s.op('dve', lambda e, b=b, dst=dst, half=half: e.tensor_scalar(
                                    out=dst[:, b, half * 512:(half + 1) * 512], in0=dst[:, b, half * 512:(half + 1) * 512],
                                    scalar1=1.0, scalar2=None, op0=ALU.add), reads=[dst], writes=[dst])
                s.barrier()
            s.es = root if root is not None else es

        if doA:
            ada_phase([0, 1], [2], {2: gate1})

        if doA:
            with ExitStack() as esA:
                s.es = esA
                wout = s.sb('wout', [128, 8, D], BF16)
                qT = s.sb('qT', [128, 4, SEQ], BF16)
                kT = s.sb('kT', [128, 4, SEQ], BF16)
                vaug = s.sb('vaug', [128, NT, 8, 65], BF16)
                upad = s.sb('upad', [128, 4, SEQ + 30], BF16)
                l1g = s.sb('l1g', [128, D], F32)
                l1b = s.sb('l1b', [128, D], F32)
                boutb = s.sb('boutb', [128, D], F32)
                clg = s.sb('clg', [128, 512], F32)
                clb = s.sb('clb', [128, 512], F32)
                bvb = s.sb('bvb', [128, 512], F32)
                cw = s.sb('cw', [128, 4, 31], F32)
                cb = s.sb('cb', [128, 4], F32)
                binfm = s.sb('binfm', [128, 20], F32)
                EB = s.sb('EB', [128, 8, NSLOT * 128], BF16)
                with ExitStack() as esE:
                    s.es = esE
                    ebsts = [s.sb('ebst0', [128, NSLOT * 128], F32), s.sb('ebst1', [128, NSLOT * 128], F32)]
                    for h in range(8):
                        eb_ = ebsts[h % 2]
                        s.dma('sp', lambda e, h=h, eb_=eb_: e.dma_start(out=eb_[:], in_=ebt_d[:, h, :]), eb_, writes=[eb_])
                        s.op('act', lambda e, h=h, eb_=eb_: e.activation(out=EB[:, h, :], in_=eb_[:], func=AF.Exp),
                             reads=[eb_], writes=[EB])
                    s.barrier()
                s.es = esA
                for k in range(8):
                    for hh in range(2):
                        s.dma('pool', lambda e, k=k, hh=hh: e.dma_start(
                            out=wout[:, k, hh * 512:(hh + 1) * 512], in_=wout_d[k * 128:(k + 1) * 128, hh * 512:(hh + 1) * 512]),
                            wout, writes=[wout])
                for dst, src in ((l1g, l1g_d), (l1b, l1b_d), (boutb, bout_d), (clg, clg_d), (clb, clb_d)):
                    s.dma('sp', lambda e, dst=dst, src=src: e.dma_start(out=dst[:], in_=bc(src)), dst, writes=[dst])
                s.dma('sp', lambda e: e.dma_start(out=bvb[:], in_=bc(bin_d[:, 1024:1536])), bvb, writes=[bvb])
                s.dma('sp', lambda e: e.dma_start(out=cw[:], in_=cw_d), cw, writes=[cw])
                s.dma('sp', lambda e: e.dma_start(out=cb[:], in_=cb_d), cb, writes=[cb])
                s.dma('sp', lambda e: e.dma_start(out=binfm[:], in_=binfm_d), binfm, writes=[binfm])
                s.op('pool', lambda e: e.memset(vaug[:], 1.0), writes=[vaug])
                s.op('pool', lambda e: e.memset(upad[:], 0.0), writes=[upad])

                for b in range(NB):
                    with ExitStack() as es1:
                        s.es = es1
                        win = s.sb('win', [128, 8, 2560], BF16)
                        xt = [s.sb('xt0', [128, D], F32), s.sb('xt1', [128, D], F32)]
                        xn = s.sb('xn', [128, D], BF16)
                        hTs = [s.sb('hT0', [128, 8, 512], BF16), s.sb('hT1', [128, 8, 512], BF16)]
                        sig = s.sb('sig', [128, 512], F32)
                        for k in range(8):
                            for cblk in range(5):
                                s.dma('pool', lambda e, k=k, cblk=cblk: e.dma_start(
                                    out=win[:, k, cblk * 512:(cblk + 1) * 512],
                                    in_=win_d[k * 128:(k + 1) * 128, cblk * 512:(cblk + 1) * 512]), win, writes=[win])
                        ziv = [0]

                        def lnchain(stile):
                            hT = hTs[stile % 2]
                            for j in range(4):
                                i = stile * 4 + j
                                x_ = xt[i % 2]
                                s.dma('sp', lambda e, x_=x_, i=i: e.dma_start(out=x_[:], in_=x_d[b, i * 128:(i + 1) * 128, :]),
                                      x_, writes=[x_])
                                ln_stats(x_, [x_[:, 0:512], x_[:, 512:1024]], 'a1', st_t, mv_t, rstd_t, nmr_t)
                                s.op('act', lambda e, x_=x_: e.activation(out=xn[:], in_=x_[:], func=AF.Identity,
                                                                         bias=nmr_t[:], scale=rstd_t[:]),
                                     reads=[x_, nmr_t, rstd_t], writes=[xn])
                                for k in range(8):
                                    s.op('pe', lambda e, k=k: e.transpose(PS_T[:, k * 128:(k + 1) * 128],
                                                                          xn[:, k * 128:(k + 1) * 128], identb[:]),
                                         reads=[xn, identb], writes=[PS_T])
                                for k in range(8):
                                    s.op('dve', lambda e, k=k, j=j: e.tensor_scalar(
                                        out=hT[:, k, j * 128:(j + 1) * 128], in0=PS_T[:, k * 128:(k + 1) * 128],
                                        scalar1=modfm[:, 8 + k, b:b + 1], scalar2=modfm[:, k, b:b + 1],
                                        op0=ALU.mult, op1=ALU.add), reads=[PS_T, modfm], writes=[hT])

                        def proj(stile):
                            hT = hTs[stile % 2]
                            t0 = stile * 512
                            order = [('q', c) for c in range(4)] + [('k', c) for c in range(4)]
                            for c in range(4):
                                order += [('cg', c), ('ca', c)]
                            for kind, c in order:
                                col = {'q': 0, 'k': 512, 'ca': 1536, 'cg': 2048}[kind] + c * 128
                                ch = col // 128
                                P = PS_S[ziv[0] % 2]
                                ziv[0] += 1
                                for k in range(8):
                                    s.op('pe', lambda e, k=k, col=col, P=P: e.matmul(
                                        P[:, 0:512], lhsT=win[:, k, col:col + 128], rhs=hT[:, k, :],
                                        start=(k == 0), stop=(k == 7)), reads=[win, hT], writes=[P])
                                if kind in ('q', 'k'):
                                    dst = qT if kind == 'q' else kT
                                    s.op('act', lambda e, P=P, dst=dst, c=c, ch=ch: e.activation(
                                        out=dst[:, c, t0:t0 + 512], in_=P[:, 0:512], func=AF.Identity,
                                        bias=binfm[:, ch:ch + 1], scale=1.0), reads=[P, binfm], writes=[dst])
                                elif kind == 'cg':
                                    s.op('act', lambda e, P=P, ch=ch: e.activation(
                                        out=sig[:], in_=P[:, 0:512], func=AF.Sigmoid, bias=binfm[:, ch:ch + 1], scale=1.0),
                                        reads=[P, binfm], writes=[sig])
                                else:
                                    s.op('dve', lambda e, P=P, c=c, ch=ch: e.scalar_tensor_tensor(
                                        out=upad[:, c, 15 + t0:15 + t0 + 512], in0=P[:, 0:512], scalar=binfm[:, ch:ch + 1],
                                        in1=sig[:], op0=ALU.add, op1=ALU.mult), reads=[P, binfm, sig], writes=[upad])
                            for j in range(4):
                                i = stile * 4 + j
                                P = PS_S[ziv[0] % 2]
                                ziv[0] += 1
                                for k in range(8):
                                    s.op('pe', lambda e, k=k, j=j, P=P: e.matmul(
                                        P[:, 0:512], lhsT=hT[:, k, j * 128:(j + 1) * 128], rhs=win[:, k, 1024:1536],
                                        start=(k == 0), stop=(k == 7)), reads=[win, hT], writes=[P])
                                s.op('dve', lambda e, P=P, i=i: e.tensor_tensor(
                                    out=vaug[:, i, :, 0:64], in0=P[:, 0:512].rearrange("p (h d) -> p h d", d=64),
                                    in1=bvb[:].rearrange("p (h d) -> p h d", d=64), op=ALU.add),
                                    reads=[P, bvb], writes=[vaug])

                        lnchain(0)
                        for stile in range(4):
                            if stile + 1 < 4:
                                lnchain(stile + 1)
                            proj(stile)
                        s.barrier()
                    s.es = esA
                    with ExitStack() as es2:
                        s.es = es2
                        caccs2 = [[s.sb('cacc%d_%d' % (q, c), [128, 512], F32) for c in range(4)] for q in range(2)]
                        Eb = [s.sb('E0', [128, 640], F32), s.sb('E1', [128, 640], F32)]
                        PT = [s.sb('PT0', [128, 640], BF16), s.sb('PT1', [128, 640], BF16)]
                        ymixs = [s.sb('ymix0', [128, D], BF16), s.sb('ymix1', [128, D], BF16)]
                        yT = s.sb('yT', [128, 8, 128], BF16)
                        cn = s.sb('cn', [128, 512], F32)
                        rden = s.sb('rden', [128, 8], F32)
                        t1 = s.sb('t1', [128, D], F32)
                        x1t = [s.sb('x1t0', [128, D], F32), s.sb('x1t1', [128, D], F32)]
                        xr = [s.sb('xr0', [128, D], F32), s.sb('xr1', [128, D], F32)]
                        hcv = [0]

                        def conv(stile):
                            caccs = caccs2[stile % 2]
                            t0 = stile * 512
                            for c in range(4):
                                s.op('dve', lambda e, c=c: e.tensor_scalar(
                                    out=caccs[c][:], in0=upad[:, c, t0:t0 + 512], scalar1=cw[:, c, 0:1], scalar2=cb[:, c:c + 1],
                                    op0=ALU.mult, op1=ALU.add), reads=[upad, cw, cb], writes=[caccs[c]])
                            for j in range(1, 31):
                                for c in range(4):
                                    s.op('dve', lambda e, c=c, j=j: e.scalar_tensor_tensor(
                                        out=caccs[c][:], in0=upad[:, c, t0 + j:t0 + j + 512], scalar=cw[:, c, j:j + 1],
                                        in1=caccs[c][:], op0=ALU.mult, op1=ALU.add), reads=[upad, cw, caccs[c]], writes=[caccs[c]])

                        def attn(i):
                            ymix = ymixs[i % 2]
                            x_ = xr[i % 2]
                            s.dma('sp', lambda e, x_=x_, i=i: e.dma_start(out=x_[:], in_=x_d[b, i * 128:(i + 1) * 128, :]),
                                  x_, writes=[x_])
                            if i == 0:
                                kts, slot0 = [0, 1, 2, 3], 3
                            elif i == 1:
                                kts, slot0 = [0, 1, 2, 3], 2
                            elif i == NT - 2:
                                kts, slot0 = [12, 13, 14, 15], 1
                            elif i == NT - 1:
                                kts, slot0 = [12, 13, 14, 15], 0
                            else:
                                kts, slot0 = [i - 2, i - 1, i, i + 1, i + 2], 7
                            nch = len(kts)
                            for h in range(8):
                                c, pb = h // 2, (h % 2) * 64
                                Sp = PS_S[hcv[0] % 2]
                                E_ = Eb[hcv[0] % 2]
                                P_ = PT[hcv[0] % 2]
                                hcv[0] += 1
                                for ci, kt in enumerate(kts):
                                    s.op('pe', lambda e, ci=ci, kt=kt, c=c, pb=pb, Sp=Sp: e.matmul(
                                        Sp[:, ci * 128:(ci + 1) * 128], lhsT=kT[pb:pb + 64, c, kt * 128:(kt + 1) * 128],
                                        rhs=qT[pb:pb + 64, c, i * 128:(i + 1) * 128], start=True, stop=True),
                                        reads=[kT, qT], writes=[Sp])
                                s.op('act', lambda e, Sp=Sp, E_=E_: e.activation(
                                    out=E_[:, 0:nch * 128], in_=Sp[:, 0:nch * 128], func=AF.Exp, scale=0.125),
                                    reads=[Sp], writes=[E_])
                                s.op('dve', lambda e, E_=E_, P_=P_, h=h: e.tensor_tensor(
                                    out=P_[:, 0:nch * 128], in0=E_[:, 0:nch * 128],
                                    in1=EB[:, h, slot0 * 128:(slot0 + nch) * 128], op=ALU.mult),
                                    reads=[E_, EB], writes=[P_])
                                Po = PS_O[h // 4]
                                o0 = (h % 4) * 65
                                for ci, kt in enumerate(kts):
                                    s.op('pe', lambda e, ci=ci, kt=kt, P_=P_, Po=Po, o0=o0, h=h: e.matmul(
                                        Po[:, o0:o0 + 65], lhsT=P_[:, ci * 128:(ci + 1) * 128], rhs=vaug[:, kt, h, :],
                                        start=(ci == 0), stop=(ci == nch - 1)), reads=[P_, vaug], writes=[Po])
                            for g in range(2):
                                Po = PS_O[g]
                                ov = Po[:, 0:260].rearrange("p (h e) -> p h e", e=65)
                                s.op('dve', lambda e, g=g, ov=ov: e.reciprocal(out=rden[:, g * 4:(g + 1) * 4], in_=ov[:, :, 64]),
                                     reads=[Po], writes=[rden])
                                s.op('dve', lambda e, g=g, ov=ov: e.tensor_tensor(
                                    out=ymix[:, g * 256:(g + 1) * 256].rearrange("p (h d) -> p h d", d=64),
                                    in0=ov[:, :, 0:64],
                                    in1=rden[:, g * 4:(g + 1) * 4][:, :, None].broadcast_to([128, 4, 64]), op=ALU.mult),
                                    reads=[Po, rden], writes=[ymix])

                        def tail(i):
                            j = i % 4
                            caccs = caccs2[(i // 4) % 2]
                            ymix = ymixs[i % 2]
                            x_ = xr[i % 2]
                            for c in range(4):
                                s.op('pe', lambda e, c=c, j=j: e.transpose(
                                    PS_M[:, c * 128:(c + 1) * 128], caccs[c][:, j * 128:(j + 1) * 128], identf[:]),
                                    reads=[caccs[c], identf], writes=[PS_M])
                            ln_stats(PS_M, [PS_M[:, 0:512]], 'cv', st_t, mv_t, rstd_t, nmr_t)
                            s.op('act', lambda e: e.activation(out=cn[:], in_=PS_M[:, :], func=AF.Identity,
                                                               bias=nmr_t[:], scale=rstd_t[:]),
                                 reads=[PS_M, nmr_t, rstd_t], writes=[cn])
                            s.op('dve', lambda e: e.tensor_tensor(out=cn[:], in0=cn[:], in1=clg[:], op=ALU.mult),
                                 reads=[cn, clg], writes=[cn])
                            s.op('pool', lambda e: e.tensor_tensor(out=cn[:], in0=cn[:], in1=clb[:], op=ALU.add),
                                 reads=[cn, clb], writes=[cn])
                            s.op('act', lambda e: e.activation(out=ymix[:, 512:1024], in_=cn[:], func=AF.Silu),
                                 reads=[cn], writes=[ymix])
                            for k in range(8):
                                s.op('pe', lambda e, k=k: e.transpose(PS_T[:, k * 128:(k + 1) * 128],
                                                                      ymix[:, k * 128:(k + 1) * 128], identb[:]),
                                     reads=[ymix, identb], writes=[PS_T])
                            s.op('act', lambda e: e.activation(out=yT[:].rearrange("p k t -> p (k t)"), in_=PS_T[:, :],
                                                               func=AF.Copy), reads=[PS_T], writes=[yT])
                            for hh in range(2):
                                for k in range(8):
                                    s.op('pe', lambda e, k=k, hh=hh: e.matmul(
                                        PS_M[:, :], lhsT=yT[:, k, :], rhs=wout[:, k, hh * 512:(hh + 1) * 512],
                                        start=(k == 0), stop=(k == 7)), reads=[yT, wout], writes=[PS_M])
                                s.op('dve', lambda e, hh=hh: e.tensor_tensor(
                                    out=t1[:, hh * 512:(hh + 1) * 512], in0=PS_M[:, :], in1=boutb[:, hh * 512:(hh + 1) * 512],
                                    op=ALU.add), reads=[PS_M, boutb], writes=[t1])
                            s.op('pool', lambda e: e.tensor_tensor(out=t1[:], in0=t1[:], in1=gate1[:, b, :], op=ALU.mult),
                                 reads=[t1, gate1], writes=[t1])
                            s.op('dve', lambda e, x_=x_: e.scalar_tensor_tensor(
                                out=t1[:], in0=x_[:], scalar=ALPHA, in1=t1[:], op0=ALU.mult, op1=ALU.add),
                                reads=[x_, t1], writes=[t1])
                            ln_stats(t1, [t1[:, 0:512], t1[:, 512:1024]], 'l1', st_t, mv_t, rstd_t, nmr_t)
                            xo = x1t[i % 2]
                            s.op('act', lambda e, xo=xo: e.activation(out=xo[:], in_=t1[:], func=AF.Identity,
                                                                     bias=nmr_t[:], scale=rstd_t[:]),
                                 reads=[t1, nmr_t, rstd_t], writes=[xo])
                            s.op('dve', lambda e, xo=xo: e.tensor_tensor(out=xo[:], in0=xo[:], in1=l1g[:], op=ALU.mult),
                                 reads=[xo, l1g], writes=[xo])
                            s.op('pool', lambda e, xo=xo: e.tensor_tensor(out=xo[:], in0=xo[:], in1=l1b[:], op=ALU.add),
                                 reads=[xo, l1b], writes=[xo])
                            s.dma('sp', lambda e, xo=xo, i=i: e.dma_start(out=x1s_d[b, i * 128:(i + 1) * 128, :], in_=xo[:]),
                                  xo, reads=[xo], writes=[X1S])
                        for i in range(NT):
                            if i % 4 == 0:
                                conv(i // 4)
                            attn(i)
                            if i >= 1:
                                tail(i - 1)
                        tail(NT - 1)
                        s.barrier()
                    s.es = esA
            s.es = es

        if doB:
            esB = es.enter_context(ExitStack())
            s.es = esB
            gate2 = s.sb('gate2', [128, NB, D], F32)
            sh2 = s.sb('sh2', [128, NB, D], F32)
            sc2 = s.sb('sc2', [128, NB, D], F32)
            with ExitStack() as esC:
                s.es = esC
                cst = [s.sb('cst%d' % r, [128, 4, 2048], BF16) for r in range(4)]
                EUVB = T(None, 'euvb')
                euv_v = euv_d.rearrange("(p r) c -> p r c", p=128)
                euvb_v = euvb_d.rearrange("(p r) c -> p r c", p=128)

                def cast_store(j):
                    c_ = cst[j % 4]
                    s.dma('pool', lambda e: e.dma_start(out=euvb_v[:, 4 * j:4 * j + 4, :], in_=c_[:]), c_, reads=[c_])

                for j in range(32):
                    c_ = cst[j % 4]
                    s.dma('pool', lambda e, c_=c_, j=j: e.dma_start(out=c_[:], in_=euv_v[:, 4 * j:4 * j + 4, :]),
                          c_, writes=[c_])
                    if j >= 1:
                        cast_store(j - 1)
                cast_store(31)
                ada_phase([], [3, 4, 5], {3: sh2, 4: sc2, 5: gate2}, root=esC)
                s.es = esC
                s.barrier()
            s.es = esB
            if True:
                wq = s.sb('wq', [128, 8, 2048], BF16)
                skT = s.sb('skT', [128, 16, 128], BF16)
                l2g = s.sb('l2g', [128, D], F32)
                l2b = s.sb('l2b', [128, D], F32)
                G = [s.sb('G%d' % r, [128, 2 * D], BF16) for r in range(RING)]
                x1t = [s.sb('bx0', [128, D], F32), s.sb('bx1', [128, D], F32)]
                h2 = [s.sb('h2a', [128, D], F32), s.sb('h2b_', [128, D], F32)]
                idx = [s.sb('idxa', [128, 128], I32), s.sb('idxb', [128, 128], I32)]
                gate = [s.sb('gatea', [128, 128], F32), s.sb('gateb', [128, 128], F32)]
                h2b = s.sb('h2b', [128, D], BF16)
                hT2 = s.sb('hT2', [128, 8, 128], BF16)
                qT2 = s.sb('qT2', [128, 16, 128], BF16)
                tmps = [s.sb('tmpa', [128, 128], F32), s.sb('tmpb', [128, 128], F32)]
                tmp2s = [s.sb('tmp2a', [128, 256], F32), s.sb('tmp2b', [128, 256], F32)]
                tv = s.sb('tv', [128, 16, 16], F32)
                ti = s.sb('ti', [128, 16, 16], U32)
                tif = s.sb('tif', [128, 16, 16], F32)
                cand = s.sb('cand', [128, 8, 16, 16], F32)
                bigs = [s.sb('biga', [128, 8, 16, 16], F32), s.sb('bigb', [128, 8, 16, 16], F32)]
                bv = s.sb('bv', [128, 8, 16], F32)
                bf = s.sb('bf', [128, 8, 16], U32)
                ais = [s.sb('aia', [128, 8, 16], U32), s.sb('aib', [128, 8, 16], U32)]
                afs = [s.sb('afa', [128, 8, 16], F32), s.sb('afb', [128, 8, 16], F32)]
                i1 = s.sb('i1', [128, 8, 16], F32)
                i2 = s.sb('i2', [128, 8, 16], F32)
                eg = s.sb('eg', [128, 8, 16], F32)
                zs = s.sb('zs', [128, 8], F32)
                actv = s.sb('actv', [128, 128], F32)
                gl = s.sb('gl', [128, 128], F32)
                wcol = s.sb('wcol', [128, 128], F32)
                tvw = [T(tv.t, 'tvw0'), T(tv.t, 'tvw1')]
                tiw = [T(ti.t, 'tiw0'), T(ti.t, 'tiw1')]
                bvw = [T(bv.t, 'bvw0'), T(bv.t, 'bvw1')]
                bfw = [T(bf.t, 'bfw0'), T(bf.t, 'bfw1')]
                actv_w = [T(actv.t, 'actv_w%d' % k) for k in range(4)]
                gl_w = [T(gl.t, 'gl_w%d' % k) for k in range(4)]
                wcol_w = [T(wcol.t, 'wcol_w%d' % k) for k in range(4)]
                dg = [s.sb('dg%d' % r, [128, 128], BF16) for r in range(3)]
                junk = s.sb('junk', [128, D], BF16)
                acc = s.sb('acc', [128, D], F32)
                ot = [s.sb('ot0', [128, D], F32), s.sb('ot1', [128, D], F32)]
                for k in range(8):
                    for cblk in range(4):
                        s.dma('pool', lambda e, k=k, cblk=cblk: e.dma_start(
                            out=wq[:, k, cblk * 512:(cblk + 1) * 512],
                            in_=wq_d[k * 128:(k + 1) * 128, cblk * 512:(cblk + 1) * 512]), wq, writes=[wq])
                for g4 in range(4):
                    s.dma('pool', lambda e, g4=g4: e.dma_start(out=skT[:, g4 * 4:(g4 + 1) * 4, :], in_=skT_d[:, g4 * 4:(g4 + 1) * 4, :]),
                          skT, writes=[skT])
                s.dma('sp', lambda e: e.dma_start(out=l2g[:], in_=bc(l2g_d)), l2g, writes=[l2g])
                s.dma('sp', lambda e: e.dma_start(out=l2b[:], in_=bc(l2b_d)), l2b, writes=[l2b])
                st_b = s.sb('st_b', [128, 2, 6], F32)
                mv_b = s.sb('mv_b', [128, 2], F32)
                rstd_b = s.sb('rstd_b', [128, 1], F32)
                nmr_b = s.sb('nmr_b', [128, 1], F32)
                PSC = PS_S[0]
                PACC = PS_S[1]

                def route(b, i, p):
                    x_, h2_, idx_, gate_ = x1t[p], h2[p], idx[p], gate[p]
                    s.dma('sp', lambda e: e.dma_start(out=x_[:], in_=x1s_d[b, i * 128:(i + 1) * 128, :]),
                          x_, reads=[X1S], writes=[x_])
                    yield 3
                    for ci_, ap_ in enumerate([x_[:, 0:512], x_[:, 512:1024]]):
                        s.op('dve', lambda e, ci_=ci_, ap_=ap_: e.bn_stats(out=st_b[:, ci_, :], in_=ap_), reads=[x_], writes=[st_b])
                    s.op('dve', lambda e: e.bn_aggr(out=mv_b[:], in_=st_b[:, 0:2, :]), reads=[st_b], writes=[mv_b])
                    s.op('act', lambda e: e.activation(out=rstd_b[:], in_=mv_b[:, 1:2], func=AF.Sqrt, bias=epst[:], scale=1.0),
                         reads=[mv_b, epst], writes=[rstd_b])
                    yield 2
                    s.op('dve', lambda e: e.reciprocal(out=rstd_b[:], in_=rstd_b[:]), reads=[rstd_b], writes=[rstd_b])
                    yield 1
                    s.op('dve', lambda e: e.tensor_scalar(out=nmr_b[:], in0=mv_b[:, 0:1], scalar1=-1.0, scalar2=rstd_b[:],
                                                           op0=ALU.mult, op1=ALU.mult), reads=[mv_b, rstd_b], writes=[nmr_b])
                    s.op('act', lambda e: e.activation(out=h2_[:], in_=x_[:], func=AF.Identity, bias=nmr_b[:], scale=rstd_b[:]),
                         reads=[x_, nmr_b, rstd_b], writes=[h2_])
                    yield 2
                    s.op('dve', lambda e: e.tensor_tensor(out=h2_[:], in0=h2_[:], in1=sc2[:, b, :], op=ALU.mult),
                         reads=[h2_, sc2], writes=[h2_])
                    yield
                    s.op('dve', lambda e: e.tensor_tensor(out=h2_[:], in0=h2_[:], in1=sh2[:, b, :], op=ALU.add),
                         reads=[h2_, sh2], writes=[h2_])
                    yield
                    s.op('act', lambda e: e.activation(out=h2b[:], in_=h2_[:], func=AF.Copy), reads=[h2_], writes=[h2b])
                    for k in range(8):
                        s.op('pe', lambda e, k=k: e.transpose(PS_T[:, k * 128:(k + 1) * 128], h2b[:, k * 128:(k + 1) * 128], identb[:]),
                             reads=[h2b, identb], writes=[PS_T])
                    yield
                    s.op('act', lambda e: e.activation(out=hT2[:].rearrange("p k t -> p (k t)"), in_=PS_T[:, :], func=AF.Copy),
                         reads=[PS_T], writes=[hT2])
                    yield
                    for g4 in range(4):
                        P = PS_O[g4 % 2]
                        for cc in range(4):
                            ch = g4 * 4 + cc
                            for k in range(8):
                                s.op('pe', lambda e, k=k, ch=ch, cc=cc, P=P: e.matmul(
                                    P[:, cc * 128:(cc + 1) * 128], lhsT=wq[:, k, ch * 128:(ch + 1) * 128], rhs=hT2[:, k, :],
                                    start=(k == 0), stop=(k == 7)), reads=[wq, hT2], writes=[P])
                            yield
                        s.op('act', lambda e, g4=g4, P=P: e.activation(
                            out=qT2[:, g4 * 4:(g4 + 1) * 4, :].rearrange("p c t -> p (c t)"), in_=P[:, :], func=AF.Copy),
                            reads=[P], writes=[qT2])
                        yield
                    for half8 in range(2):
                        for q8 in range(8):
                            hp = half8 * 8 + q8
                            s.op('pe', lambda e, hp=hp, q8=q8: e.matmul(
                                PSC[:, q8 * 128:(q8 + 1) * 128], lhsT=qT2[:, hp, :], rhs=skT[:, hp, :], start=True, stop=True),
                                reads=[qT2, skT], writes=[PSC])
                        yield (16 if half8 == 0 else 3)
                        for q8 in range(0, 8, 2):
                            hps = [half8 * 8 + q8, half8 * 8 + q8 + 1]
                            srcs = [PSC[:, q8 * 128:(q8 + 1) * 128], PSC[:, (q8 + 1) * 128:(q8 + 2) * 128]]
                            for hp, src in zip(hps, srcs):
                                s.op('dve', lambda e, hp=hp, src=src: e.max(out=tv[:, hp, 0:8], in_=src), reads=[PSC], writes=[tvw[hp % 2]])
                                yield
                            for hp, src, tm in zip(hps, srcs, tmps):
                                s.op('dve', lambda e, hp=hp, src=src, tm=tm: e.match_replace(
                                    out=tm[:], in_to_replace=tv[:, hp, 0:8], in_values=src, imm_value=-1e30),
                                    reads=[PSC, tvw[hp % 2]], writes=[tm])
                                yield
                            for hp, tm in zip(hps, tmps):
                                s.op('dve', lambda e, hp=hp, tm=tm: e.max(out=tv[:, hp, 8:16], in_=tm[:]), reads=[tm], writes=[tvw[hp % 2]])
                                yield
                            for hp, src in zip(hps, srcs):
                                s.op('dve', lambda e, hp=hp, src=src: e.max_index(out=ti[:, hp, 0:8], in_max=tv[:, hp, 0:8],
                                                                                 in_values=src), reads=[PSC, tvw[hp % 2]], writes=[tiw[hp % 2]])
                                yield
                            for hp, tm in zip(hps, tmps):
                                s.op('dve', lambda e, hp=hp, tm=tm: e.max_index(out=ti[:, hp, 8:16], in_max=tv[:, hp, 8:16],
                                                                               in_values=tm[:]), reads=[tm, tvw[hp % 2]], writes=[tiw[hp % 2]])
                                yield
                    tv4 = tv[:].rearrange("p (h t) k -> p h t k", t=2)
                    s.op('dve', lambda e: e.tensor_tensor(
                        out=cand[:], in0=tv4[:, :, 0, :][:, :, :, None].broadcast_to([128, 8, 16, 16]),
                        in1=tv4[:, :, 1, :][:, :, None, :].broadcast_to([128, 8, 16, 16]), op=ALU.add),
                        reads=tvw, writes=[cand])
                    yield
                    for h0 in range(0, 8, 2):
                        hs = [h0, h0 + 1]
                        cvs = [cand[:, h, :, :].rearrange("p a b -> p (a b)") for h in hs]
                        for h, cv in zip(hs, cvs):
                            s.op('dve', lambda e, h=h, cv=cv: e.max(out=bv[:, h, 0:8], in_=cv), reads=[cand], writes=[bvw[h % 2]])
                            yield
                        for h, cv, tm in zip(hs, cvs, tmp2s):
                            s.op('dve', lambda e, h=h, cv=cv, tm=tm: e.match_replace(
                                out=tm[:], in_to_replace=bv[:, h, 0:8], in_values=cv, imm_value=-1e30),
                                reads=[cand, bvw[h % 2]], writes=[tm])
                            yield
                        for h, tm in zip(hs, tmp2s):
                            s.op('dve', lambda e, h=h, tm=tm: e.max(out=bv[:, h, 8:16], in_=tm[:]), reads=[tm], writes=[bvw[h % 2]])
                            yield
                        for h, cv in zip(hs, cvs):
                            s.op('dve', lambda e, h=h, cv=cv: e.max_index(out=bf[:, h, 0:8], in_max=bv[:, h, 0:8], in_values=cv),
                                 reads=[cand, bvw[h % 2]], writes=[bfw[h % 2]])
                            yield
                        for h, tm in zip(hs, tmp2s):
                            s.op('dve', lambda e, h=h, tm=tm: e.max_index(out=bf[:, h, 8:16], in_max=bv[:, h, 8:16], in_values=tm[:]),
                                 reads=[tm, bvw[h % 2]], writes=[bfw[h % 2]])
                            yield
                    s.op('dve', lambda e: e.tensor_copy(out=tif[:], in_=ti[:]), reads=tiw, writes=[tif])
                    yield
                    tif4 = tif[:].rearrange("p (h t) k -> p h t k", t=2)
                    s.op('dve', lambda e: e.tensor_scalar(out=ais[0][:], in0=bf[:], scalar1=4, scalar2=None,
                                                           op0=ALU.logical_shift_right), reads=bfw, writes=[ais[0]])
                    s.op('dve', lambda e: e.tensor_scalar(out=ais[1][:], in0=bf[:], scalar1=15, scalar2=None,
                                                           op0=ALU.bitwise_and), reads=bfw, writes=[ais[1]])
                    yield
                    for half in range(2):
                        s.op('dve', lambda e, half=half: e.tensor_copy(out=afs[half][:], in_=ais[half][:]),
                             reads=[ais[half]], writes=[afs[half]])
                    yield
                    for half in range(2):
                        s.op('dve', lambda e, half=half: e.tensor_tensor(
                            out=bigs[half][:], in0=afs[half][:][:, :, :, None].broadcast_to([128, 8, 16, 16]),
                            in1=iota16[:][:, None, None, :].broadcast_to([128, 8, 16, 16]), op=ALU.is_equal),
                            reads=[afs[half], iota16], writes=[bigs[half]])
                        yield
                    for half in range(2):
                        s.op('dve', lambda e, half=half: e.tensor_tensor(
                            out=bigs[half][:], in0=bigs[half][:],
                            in1=tif4[:, :, half, :][:, :, None, :].broadcast_to([128, 8, 16, 16]),
                            op=ALU.mult), reads=[bigs[half], tif], writes=[bigs[half]])
                        yield
                    for half, dst in ((0, i1), (1, i2)):
                        s.op('dve', lambda e, half=half, dst=dst: e.tensor_reduce(out=dst[:], in_=bigs[half][:], axis=AX.X, op=ALU.add),
                             reads=[bigs[half]], writes=[dst])
                        yield
                    s.op('dve', lambda e: e.scalar_tensor_tensor(
                        out=idx_[:].rearrange("p (h k) -> p h k", k=16), in0=i1[:], scalar=128.0, in1=i2[:],
                        op0=ALU.mult, op1=ALU.add), reads=[i1, i2], writes=[idx_])
                    s.op('dve', lambda e: e.tensor_tensor(out=eg[:], in0=bv[:], in1=bv[:, :, 0:1].broadcast_to([128, 8, 16]),
                                                           op=ALU.subtract), reads=bvw, writes=[eg])
                    yield
                    s.op('act', lambda e: e.activation(out=eg[:], in_=eg[:], func=AF.Exp), reads=[eg], writes=[eg])
                    yield 2
                    s.op('dve', lambda e: e.tensor_reduce(out=zs[:], in_=eg[:], axis=AX.X, op=ALU.add), reads=[eg], writes=[zs])
                    yield 1
                    s.op('dve', lambda e: e.reciprocal(out=zs[:], in_=zs[:]), reads=[zs], writes=[zs])
                    s.op('dve', lambda e: e.tensor_tensor(
                        out=gate_[:].rearrange("p (h k) -> p h k", k=16), in0=eg[:],
                        in1=zs[:][:, :, None].broadcast_to([128, 8, 16]), op=ALU.mult), reads=[eg, zs], writes=[gate_])
                    yield

                idle = [0]

                def drain(g, n=None):
                    if g is None:
                        return None
                    if n is not None and idle[0] > 0:
                        idle[0] -= 1
                        return g
                    k = 0
                    for v in g:
                        k += 1
                        if n is not None and v:
                            idle[0] = int(v) - 1
                            return g
                        if n is not None and k >= n:
                            return g
                    return None

                tiles = [(b, i) for b in range(1 if (dbg or b1) else NB) for i in range(nb1)]
                drain(route(tiles[0][0], tiles[0][1], 0))
                gi = 0
                for tn, (b, i) in enumerate(tiles):
                    p = tn % 2
                    x_, h2_, idx_, gate_ = x1t[p], h2[p], idx[p], gate[p]
                    if dbg:
                        s.barrier()
                        for dd, tt, vw in ((dbg_idx, idx_, idx_[:]), (dbg_gate, gate_, gate_[:]), (dbg_h2, h2_, h2_[:]),
                                           (dbg_tv, tv, tv[:].rearrange("p a b -> p (a b)")),
                                           (dbg_ti, ti, ti[:].rearrange("p a b -> p (a b)")),
                                           (dbg_bv, bv, bv[:].rearrange("p a b -> p (a b)")),
                                           (dbg_bf, bf, bf[:].rearrange("p a b -> p (a b)"))):
                            s.dma('sp', lambda e, dd=dd, vw=vw: e.dma_start(out=dd, in_=vw), tt, reads=[tt])
                        break
                    nxt = route(tiles[tn + 1][0], tiles[tn + 1][1], 1 - p) if tn + 1 < len(tiles) else None

                    def vstep(sl, g_):
                        d_ = dg[sl % 3]
                        s.op('act', lambda e: e.activation(out=wcol[:, sl:sl + 1], in_=gl[:, sl:sl + 1], func=AF.Copy,
                                                           scale=gate_[:, sl:sl + 1]), reads=[gl_w[sl % 4], gate_], writes=[wcol_w[sl % 4]])
                        s.op('act', lambda e: e.activation(out=d_[:], in_=identb[:], func=AF.Copy, scale=wcol[:, sl:sl + 1]),
                             reads=[identb, wcol_w[sl % 4]], writes=[d_])
                        for hh in range(2):
                            s.op('pe', lambda e, hh=hh: e.matmul(
                                PACC[:, hh * 512:(hh + 1) * 512], lhsT=d_[:], rhs=g_[:, D + hh * 512:D + (hh + 1) * 512],
                                start=(sl == 0), stop=(sl == 127)), reads=[d_, g_], writes=[PACC])

                    prev = None
                    for sl in range(128):
                        g_ = G[gi % RING]
                        gi += 1
                        s.dma('pool', lambda e, g_=g_, sl=sl: e.indirect_dma_start(
                            out=g_[:], out_offset=None, in_=euvb_d,
                            in_offset=bass.IndirectOffsetOnAxis(ap=idx_[:, sl:sl + 1], axis=0)), g_, reads=[idx_], writes=[g_])
                        s.op('dve', lambda e, g_=g_, sl=sl: e.scalar_tensor_tensor(
                            out=junk[:], in0=g_[:, 0:D], scalar=1.0, in1=h2_[:], op0=ALU.mult, op1=ALU.mult,
                            accum_out=actv[:, sl:sl + 1]), reads=[g_, h2_], writes=[actv_w[sl % 4]])
                        if prev is not None:
                            vstep(*prev)
                        s.op('act', lambda e, sl=sl: e.activation(out=gl[:, sl:sl + 1], in_=actv[:, sl:sl + 1], func=AF.Gelu),
                             reads=[actv_w[sl % 4]], writes=[gl_w[sl % 4]])
                        prev = (sl, g_)
                        nxt = drain(nxt, 3)
                    vstep(*prev)
                    drain(nxt)
                    for hh in range(2):
                        s.op('dve', lambda e, hh=hh: e.tensor_tensor(
                            out=acc[:, hh * 512:(hh + 1) * 512], in0=PACC[:, hh * 512:(hh + 1) * 512],
                            in1=gate2[:, b, hh * 512:(hh + 1) * 512], op=ALU.mult), reads=[PACC, gate2], writes=[acc])
                    s.op('dve', lambda e, x_=x_: e.scalar_tensor_tensor(
                        out=acc[:], in0=x_[:], scalar=ALPHA, in1=acc[:], op0=ALU.mult, op1=ALU.add),
                        reads=[x_, acc], writes=[acc])
                    ln_stats(acc, [acc[:, 0:512], acc[:, 512:1024]], 'l2', st_t, mv_t, rstd_t, nmr_t)
                    o_ = ot[tn % 2]
                    s.op('act', lambda e, o_=o_: e.activation(out=o_[:], in_=acc[:], func=AF.Identity,
                                                             bias=nmr_t[:], scale=rstd_t[:]),
                         reads=[acc, nmr_t, rstd_t], writes=[o_])
                    s.op('dve', lambda e, o_=o_: e.tensor_tensor(out=o_[:], in0=o_[:], in1=l2g[:], op=ALU.mult),
                         reads=[o_, l2g], writes=[o_])
                    s.op('dve', lambda e, o_=o_: e.tensor_tensor(out=o_[:], in0=o_[:], in1=l2b[:], op=ALU.add),
                         reads=[o_, l2b], writes=[o_])
                    s.dma('sp', lambda e, o_=o_, i=i, b=b: e.dma_start(out=out_d[b, i * 128:(i + 1) * 128, :], in_=o_[:]),
                          o_, reads=[o_])
                s.barrier()
            s.es = es
        s.barrier()
    return nc


def _bias_table(rpb):
    specs = [(d, 'F') for d in (-6, -4, -2, 0, 2, 4, 6)] + [(-4, 'I'), (-2, 'F'), (0, 'F'), (2, 'F'), (4, 'I')]
    kk = np.arange(128)
    kro, kc = kk // 64, kk % 64
    qro, qc = kk // 64, kk % 64
    cs = np.clip(qc - 8, 0, 48)
    colv = (kc[:, None] >= cs[None, :]) & (kc[:, None] < cs[None, :] + 16)
    dc = np.clip(kc[:, None] - qc[None, :] + 15, 0, 30)
    tab = np.empty((8, NSLOT, 128, 128), np.float32)
    for si, (dl, kind) in enumerate(specs):
        dr = dl + kro[:, None] - qro[None, :]
        valid = colv.copy()
        if kind == 'I':
            valid &= (dr >= -4) & (dr <= 3)
        dri = np.clip(dr + 7, 0, 14)
        vals = rpb[:, dri, dc]
        tab[:, si] = np.where(valid[None], vals, np.float32(NEG))
    return np.ascontiguousarray(tab.transpose(2, 0, 1, 3).reshape(128, 8, NSLOT * 128))


def make_in_maps(inp):
    f = lambda a: np.ascontiguousarray(np.asarray(a, dtype=np.float32))
    x = f(inp["x"])
    c = f(inp["c"])
    shared = {
        "w_ada": f(inp["w_ada"][0]),
        "b_ada": f(inp["b_ada"][0]).reshape(1, -1),
        "b_ada_fm": f(f(inp["b_ada"][0]).reshape(48, 128).T),
        "w_in": f(inp["w_in"][0]),
        "b_in_fm": f(f(inp["b_in"][0]).reshape(20, 128).T),
        "b_in": f(inp["b_in"][0]).reshape(1, -1),
        "attn_bias": _bias_table(f(inp["rel_pos_bias"][0])),
        "conv_w_fm": f(f(inp["conv_w"][0]).reshape(31, 4, 128).transpose(2, 1, 0)),
        "conv_b_fm": f(f(inp["conv_b"][0]).reshape(4, 128).T),
        "conv_ln_g": f(inp["conv_ln_g"][0]).reshape(1, -1),
        "conv_ln_b": f(inp["conv_ln_b"][0]).reshape(1, -1),
        "w_out": f(inp["w_out"][0]),
        "b_out": f(inp["b_out"][0]).reshape(1, -1),
        "ln1_g": f(inp["ln1_g"][0]).reshape(1, -1),
        "ln1_b": f(inp["ln1_b"][0]).reshape(1, -1),
        "w_query": f(inp["w_query"][0]),
        "skT": f(f(inp["sub_keys"][0]).reshape(16, 128, 128).transpose(2, 0, 1)),
        "euv": np.ascontiguousarray(np.concatenate([f(inp["expert_u"][0]), f(inp["expert_v"][0])], axis=1)),
        "ln2_g": f(inp["ln2_g"][0]).reshape(1, -1),
        "ln2_b": f(inp["ln2_b"][0]).reshape(1, -1),
        "ident": np.eye(128, dtype=np.float32),
        "iota16": np.ascontiguousarray(np.broadcast_to(np.arange(16, dtype=np.float32), (128, 16))),
    }
    maps = []
    for core in range(NCORES):
        m = dict(shared)
        m["x"] = np.ascontiguousarray(x[NB * core:NB * (core + 1)])
        m["cT"] = np.ascontiguousarray(c[NB * core:NB * (core + 1)].reshape(NB, 8, 128).transpose(2, 1, 0))
        maps.append(m)
    return maps


_NC_CACHE = {}


def kernel(**inputs):
    if 'full' not in _NC_CACHE:
        _NC_CACHE['full'] = build('full')
    nc = _NC_CACHE['full']
    maps = make_in_maps(inputs)
    res = run_bass_kernel_spmd(nc, maps, core_ids=list(range(NCORES)))
    out = np.concatenate([r["out"] for r in res.results], axis=0)
    return out.astype(np.float32)
```
